# Optimizing a Trainium2 kernel written in Bass

```python
import math
import jax, jax.numpy as jnp
from jax import lax
import numpy as np

D_MODEL = 1024
BATCH = 16
SEQ = 2048
DEPTH = 2

RET_HEADS = 8
RET_DK = 64
RET_DV = 128
RET_CHUNK = 128
RET_THETA = 10000.0
DSA_HEADS = 8
DSA_DIM = 64
ROT_DIM = DSA_DIM // 4
ROPE_THETA = 500000.0
IDX_HEADS = 4
IDX_DIM = 64
TOPK_MAX = 256
Q_BLOCK = 128
RWKV_HEADS = 8
RWKV_DIM = 64
DECAY_LORA = 64
AAA_LORA = 64
GATE_LORA = 128
RWKV_GN_EPS = 64e-5
D_FF = 2816
RMS_EPS = 1e-6
NEG_INF = -1e30
N_BRANCH = 3

RET_WIDTH = RET_HEADS * RET_DV
DSA_WIDTH = DSA_HEADS * DSA_DIM
RWKV_WIDTH = RWKV_HEADS * RWKV_DIM
RET_SIZES = (RET_HEADS * RET_DK, RET_HEADS * RET_DK, RET_HEADS * RET_DV, RET_HEADS * RET_DV)
DSA_SIZES = (DSA_HEADS * DSA_DIM, DSA_DIM, DSA_DIM, IDX_HEADS * IDX_DIM, IDX_DIM, IDX_HEADS)
RWKV_SIZES = (RWKV_WIDTH, RWKV_WIDTH, RWKV_WIDTH, DECAY_LORA, AAA_LORA, GATE_LORA)
GATE_COLS = N_BRANCH * D_MODEL
GROUP_SIZES = (sum(RET_SIZES), sum(DSA_SIZES), sum(RWKV_SIZES), GATE_COLS)
N_IN = sum(GROUP_SIZES)

kernel_name = 'hybrid_retention_dsa_rwkv7_macaron'


def split_cols(a, sizes):
    cuts = [int(c) for c in np.cumsum(sizes)[:-1]]
    return jnp.split(a, cuts, axis=-1)


def rms_norm(x, g):
    xf = x.astype(jnp.float32)
    y = xf * lax.rsqrt(jnp.mean(xf * xf, axis=-1, keepdims=True) + RMS_EPS)
    return (y * g.astype(jnp.float32)).astype(x.dtype)


def swiglu(h, w_in, w_out):
    gate, up = jnp.split(h @ w_in, 2, axis=-1)
    return (jax.nn.silu(gate) * up) @ w_out


def rope_tables(seq, rot_dim, theta):
    inv_freq = theta ** (-jnp.arange(0, rot_dim, 2, dtype=jnp.float32) / rot_dim)
    ang = jnp.arange(seq, dtype=jnp.float32)[:, None] * inv_freq[None, :]
    return jnp.cos(ang), jnp.sin(ang)


def apply_rope(x, cos, sin):
    xf = x.astype(jnp.float32)
    x1, x2 = jnp.split(xf, 2, axis=-1)
    return jnp.concatenate([x1 * cos - x2 * sin, x1 * sin + x2 * cos], axis=-1).astype(x.dtype)


def partial_rope(x, cos, sin):
    return jnp.concatenate([apply_rope(x[..., :ROT_DIM], cos, sin), x[..., ROT_DIM:]], axis=-1)


def token_shift(p, mu):
    prev = jnp.pad(p[:, :-1], ((0, 0), (1, 0), (0, 0)))
    return p + mu * (prev - p)


def retention(q, k, v, g, cos, sin):
    B, T = q.shape[:2]
    n_chunks = T // RET_CHUNK
    f32 = jnp.float32
    q = apply_rope(q, cos[:, None], sin[:, None]).astype(f32)
    k = apply_rope(k, cos[:, None], sin[:, None]).astype(f32) * (RET_DK ** -0.5)
    log_gamma = jnp.log(1.0 - 2.0 ** (-5.0 - jnp.arange(RET_HEADS, dtype=f32)))
    pos = jnp.arange(RET_CHUNK, dtype=f32)
    rel = pos[:, None] - pos[None, :]
    decay_mask = jnp.where(rel >= 0, jnp.exp(log_gamma[:, None, None] * jnp.maximum(rel, 0.0)), 0.0)
    q_decay = jnp.exp(log_gamma[:, None] * (pos + 1.0))[None, :, :, None]
    k_decay = jnp.exp(log_gamma[:, None] * (RET_CHUNK - 1.0 - pos))[None, :, :, None]
    chunk_decay = jnp.exp(log_gamma * RET_CHUNK)[None, :, None, None]

    def to_chunks(a):
        return a.reshape(B, n_chunks, RET_CHUNK, RET_HEADS, a.shape[-1]).transpose(1, 0, 3, 2, 4)

    def step(state, inp):
        qc, kc, vc = inp
        scores = jnp.einsum('bhid,bhjd->bhij', qc, kc) * decay_mask
        out = jnp.einsum('bhij,bhjv->bhiv', scores, vc) + jnp.einsum('bhid,bhdv->bhiv', qc, state) * q_decay
        state = state * chunk_decay + jnp.einsum('bhjd,bhjv->bhdv', kc * k_decay, vc)
        return state, out

    state0 = jnp.zeros((B, RET_HEADS, RET_DK, RET_DV), f32)
    _, o = lax.scan(step, state0, (to_chunks(q), to_chunks(k), to_chunks(v.astype(f32))))
    o = o.transpose(1, 0, 3, 2, 4).reshape(B, T, RET_HEADS, RET_DV)
    o = o * lax.rsqrt(jnp.mean(o * o, axis=-1, keepdims=True) + RMS_EPS)
    return (jax.nn.silu(g.astype(f32)) * o.reshape(B, T, RET_WIDTH)).astype(g.dtype)


def sparse_attention(q, k, v, q_idx, k_idx, w_idx, cos, sin):
    B, T = q.shape[:2]
    n_blocks = T // Q_BLOCK
    k_top = min(TOPK_MAX, T // 4)
    f32 = jnp.float32
    q = partial_rope(q, cos[:, None], sin[:, None])
    k = partial_rope(k, cos, sin)
    q_idx = partial_rope(q_idx, cos[:, None], sin[:, None]).astype(f32)
    k_idx = partial_rope(k_idx, cos, sin).astype(f32)
    w_idx = w_idx.astype(f32) * (IDX_HEADS ** -0.5)
    key_pos = jnp.arange(T, dtype=jnp.int32)
    gather = jax.vmap(lambda table, idx: table[idx])

    def to_blocks(a):
        return jnp.moveaxis(a.reshape((B, n_blocks, Q_BLOCK) + a.shape[2:]), 1, 0)

    def one_block(inp):
        qb, qib, wb, start = inp
        q_pos = start + jnp.arange(Q_BLOCK, dtype=jnp.int32)
        rel = jnp.einsum('bqhd,bkd->bqhk', qib, k_idx) * (IDX_DIM ** -0.5)
        index_score = jnp.einsum('bqh,bqhk->bqk', wb, jax.nn.relu(rel))
        causal = key_pos[None, :] <= q_pos[:, None]
        index_score = jnp.where(causal[None], index_score, NEG_INF)
        _, sel = lax.top_k(index_score, k_top)
        valid = sel <= q_pos[None, :, None]
        k_sel = gather(k, sel)
        v_sel = gather(v, sel)
        logits = jnp.einsum('bqhd,bqkd->bhqk', qb, k_sel).astype(f32) * (DSA_DIM ** -0.5)
        logits = jnp.where(valid[:, None], logits, NEG_INF)
        p = jax.nn.softmax(logits, axis=-1)
        return jnp.einsum('bhqk,bqkd->bqhd', p.astype(v.dtype), v_sel)

    starts = jnp.arange(n_blocks, dtype=jnp.int32) * Q_BLOCK
    out = lax.map(one_block, (to_blocks(q), to_blocks(q_idx), to_blocks(w_idx), starts))
    return jnp.moveaxis(out, 0, 1).reshape(B, T, DSA_WIDTH)


def rwkv7_time_mix(p, mu, w0, w2, a0, a2, g2, k_k, k_a, r_k, ln_w, ln_b):
    B, T = p.shape[:2]
    f32 = jnp.float32
    r, k, v, w_lr, a_lr, g_lr = split_cols(token_shift(p, mu), RWKV_SIZES)
    log_decay = -jax.nn.softplus(-(w0 + jnp.tanh(w_lr) @ w2).astype(f32)) - 0.5
    decay = jnp.exp(-jnp.exp(log_decay))
    a = jax.nn.sigmoid((a0 + a_lr @ a2).astype(f32))
    g = jax.nn.sigmoid(g_lr) @ g2

    def heads(t):
        return t.astype(f32).reshape(B, T, RWKV_HEADS, RWKV_DIM)

    r_h, v_h, decay_h, a_h = heads(r), heads(v), heads(decay), heads(a)
    kk = heads(k * k_k)
    kk = kk / jnp.maximum(jnp.sqrt(jnp.sum(kk * kk, axis=-1, keepdims=True)), 1e-12)
    k_h = heads(k) * (1.0 + (a_h - 1.0) * k_a.astype(f32).reshape(RWKV_HEADS, RWKV_DIM))

    def step(S, inp):
        r_t, w_t, k_t, v_t, kk_t, a_t = inp
        sa = jnp.einsum('bhvk,bhk->bhv', S, -kk_t)
        S = S * w_t[:, :, None, :] + sa[..., None] * (kk_t * a_t)[:, :, None, :] + v_t[..., None] * k_t[:, :, None, :]
        return S, jnp.einsum('bhvk,bhk->bhv', S, r_t)

    S0 = jnp.zeros((B, RWKV_HEADS, RWKV_DIM, RWKV_DIM), f32)
    xs = tuple(jnp.moveaxis(t, 1, 0) for t in (r_h, decay_h, k_h, v_h, kk, a_h))
    _, y = lax.scan(step, S0, xs)
    y = jnp.moveaxis(y, 0, 1)
    mean = jnp.mean(y, axis=-1, keepdims=True)
    var = jnp.mean(jnp.square(y - mean), axis=-1, keepdims=True)
    y = ((y - mean) * lax.rsqrt(var + RWKV_GN_EPS)).reshape(B, T, RWKV_WIDTH)
    y = y * ln_w.astype(f32) + ln_b.astype(f32)
    bonus = jnp.sum(r_h * k_h * r_k.astype(f32), axis=-1, keepdims=True) * v_h
    y = y + bonus.reshape(B, T, RWKV_WIDTH)
    return (y * g.astype(f32)).astype(p.dtype)


def hybrid_mixer(h, w_in, rwkv_mu, rwkv_w0, rwkv_w2, rwkv_a0, rwkv_a2, rwkv_g2, rwkv_k_k, rwkv_k_a,
                 rwkv_r_k, rwkv_ln_w, rwkv_ln_b, p_ret, p_dsa, p_rwkv, w_out, ret_cs, rot_cs):
    B, T, _ = h.shape
    proj = h @ w_in
    ret_p, dsa_p, rwkv_p, gate_p = split_cols(proj, GROUP_SIZES)
    rq, rk, rv, rg = split_cols(ret_p, RET_SIZES)
    y_ret = retention(rq.reshape(B, T, RET_HEADS, RET_DK), rk.reshape(B, T, RET_HEADS, RET_DK),
                      rv.reshape(B, T, RET_HEADS, RET_DV), rg, ret_cs[0], ret_cs[1])
    dq, dk, dv, qi, ki, wi = split_cols(dsa_p, DSA_SIZES)
    y_dsa = sparse_attention(dq.reshape(B, T, DSA_HEADS, DSA_DIM), dk, dv,
                             qi.reshape(B, T, IDX_HEADS, IDX_DIM), ki, wi, rot_cs[0], rot_cs[1])
    y_rwkv = rwkv7_time_mix(rwkv_p, rwkv_mu, rwkv_w0, rwkv_w2, rwkv_a0, rwkv_a2, rwkv_g2,
                            rwkv_k_k, rwkv_k_a, rwkv_r_k, rwkv_ln_w, rwkv_ln_b)
    g_ret, g_dsa, g_rwkv = jnp.split(jax.nn.sigmoid(gate_p), N_BRANCH, axis=-1)
    merged = g_ret * (y_ret @ p_ret) + g_dsa * (y_dsa @ p_dsa) + g_rwkv * (y_rwkv @ p_rwkv)
    return merged @ w_out


def setup_inputs(seed: int = 0) -> dict:
    key = jax.random.key(seed)
    ks = iter(jax.random.split(key, 32))
    L, D = DEPTH, D_MODEL

    def normal(shape, scale):
        return jax.random.normal(next(ks), shape, jnp.float32) * scale

    def gain(shape):
        return 1.0 + normal(shape, 0.02)

    decay_base = jnp.tile(jnp.linspace(-6.0, 1.0, RWKV_DIM, dtype=jnp.float32), RWKV_HEADS)
    return {
        'x': normal((BATCH, SEQ, D), 1.0),
        'norm_ffa': gain((L, D)),
        'ffa_w_in': normal((L, D, 2 * D_FF), D ** -0.5),
        'ffa_w_out': normal((L, D_FF, D), D_FF ** -0.5),
        'norm_mix': gain((L, D)),
        'w_in': normal((L, D, N_IN), D ** -0.5),
        'rwkv_mu': jax.random.uniform(next(ks), (L, sum(RWKV_SIZES)), jnp.float32, 0.05, 0.95),
        'rwkv_w0': decay_base[None, :] + normal((L, RWKV_WIDTH), 0.1),
        'rwkv_w2': normal((L, DECAY_LORA, RWKV_WIDTH), 0.1 * DECAY_LORA ** -0.5),
        'rwkv_a0': normal((L, RWKV_WIDTH), 0.1),
        'rwkv_a2': normal((L, AAA_LORA, RWKV_WIDTH), 0.5 * AAA_LORA ** -0.5),
        'rwkv_g2': normal((L, GATE_LORA, RWKV_WIDTH), GATE_LORA ** -0.5),
        'rwkv_k_k': 0.85 + normal((L, RWKV_WIDTH), 0.02),
        'rwkv_k_a': 1.0 + normal((L, RWKV_WIDTH), 0.02),
        'rwkv_r_k': normal((L, RWKV_HEADS, RWKV_DIM), 0.1),
        'rwkv_ln_w': gain((L, RWKV_WIDTH)),
        'rwkv_ln_b': normal((L, RWKV_WIDTH), 0.02),
        'p_ret': normal((L, RET_WIDTH, D), RET_WIDTH ** -0.5),
        'p_dsa': normal((L, DSA_WIDTH, D), DSA_WIDTH ** -0.5),
        'p_rwkv': normal((L, RWKV_WIDTH, D), RWKV_WIDTH ** -0.5),
        'w_out': normal((L, D, D), D ** -0.5),
        'norm_ffb': gain((L, D)),
        'ffb_w_in': normal((L, D, 2 * D_FF), D ** -0.5),
        'ffb_w_out': normal((L, D_FF, D), D_FF ** -0.5),
        'norm_final': gain((D,)),
    }


def reference(x, norm_ffa, ffa_w_in, ffa_w_out, norm_mix, w_in, rwkv_mu, rwkv_w0, rwkv_w2, rwkv_a0,
              rwkv_a2, rwkv_g2, rwkv_k_k, rwkv_k_a, rwkv_r_k, rwkv_ln_w, rwkv_ln_b, p_ret, p_dsa, p_rwkv,
              w_out, norm_ffb, ffb_w_in, ffb_w_out, norm_final):
    T = x.shape[1]
    ret_cs = rope_tables(T, RET_DK, RET_THETA)
    rot_cs = rope_tables(T, ROT_DIM, ROPE_THETA)
    for l in range(DEPTH):
        h = rms_norm(x, norm_ffa[l])
        x = x + 0.5 * swiglu(h, ffa_w_in[l], ffa_w_out[l])
        h = rms_norm(x, norm_mix[l])
        x = x + hybrid_mixer(h, w_in[l], rwkv_mu[l], rwkv_w0[l], rwkv_w2[l], rwkv_a0[l], rwkv_a2[l],
                             rwkv_g2[l], rwkv_k_k[l], rwkv_k_a[l], rwkv_r_k[l], rwkv_ln_w[l], rwkv_ln_b[l],
                             p_ret[l], p_dsa[l], p_rwkv[l], w_out[l], ret_cs, rot_cs)
        h = rms_norm(x, norm_ffb[l])
        x = x + 0.5 * swiglu(h, ffb_w_in[l], ffb_w_out[l])
    return rms_norm(x, norm_final)
```

```python
import contextlib
import numpy as np
import concourse.bass as bass
import concourse.mybir as mybir
from concourse.bass_utils import run_bass_kernel_spmd

F32 = mybir.dt.float32
BF16 = mybir.dt.bfloat16
AF = mybir.ActivationFunctionType
ALU = mybir.AluOpType
AX = mybir.AxisListType

ND = 40
ND_SP = 24


class Buf:
    __slots__ = ("name", "writers", "readers", "excl")

    def __init__(self, name=""):
        self.name = name
        self.excl = False
        self.writers = {}
        self.readers = {}

    @property
    def last_w(self):
        return dict(self.writers)

    @last_w.setter
    def last_w(self, v):
        self.writers = dict(v) if isinstance(v, dict) else ({v[0]: v[1]} if v else {})


class Tile:
    def __init__(self, t, name, nsub=1):
        self.t = t
        self.b = Buf(name)
        self.subs = [Buf(f"{name}.{i}") for i in range(nsub)] if nsub > 1 else None

    def __getitem__(self, idx):
        return self.t[idx]


class KB:
    def __init__(self, nc):
        self.nc = nc
        self.root = contextlib.ExitStack()
        self.E = {"pe": nc.tensor, "act": nc.scalar, "dve": nc.vector, "pool": nc.gpsimd, "sp": nc.sync}
        self.sem = {n: self.root.enter_context(nc.semaphore("s_" + n)) for n in self.E}
        self.cnt = {n: 0 for n in self.E}
        self.seen = {n: {} for n in self.E}
        self.dsem = [self.root.enter_context(nc.semaphore(f"d_{i}")) for i in range(ND)]
        self.dcnt = [0] * ND
        self.dnext = 0
        self.dnext_pool = ND_SP
        self.nwaits = 0
        self.ninst = 0
        self.uid = 0

    def sb(self, es, shape, dtype, name=None, nsub=1):
        self.uid += 1
        name = f"{name or 't'}_{self.uid}"
        t = es.enter_context(self.nc.sbuf_tensor(name, list(shape), dtype))
        return Tile(t, name, nsub)

    def ps(self, es, shape, dtype, name=None):
        self.uid += 1
        name = f"{name or 'p'}_{self.uid}"
        t = es.enter_context(self.nc.psum_tensor(name, list(shape), dtype))
        tl = Tile(t, name)
        tl.b.excl = True
        return tl

    def _semof(self, key):
        return self.sem[key] if isinstance(key, str) else self.dsem[key[1]]

    def _wait(self, eng, tok):
        if tok is None:
            return
        key, val = tok
        if self.seen[eng].get(key, 0) >= val:
            return
        self.E[eng].wait_ge(self._semof(key), val)
        self.seen[eng][key] = val
        self.nwaits += 1

    def _deps(self, eng, r, w, accum=False):
        for b in r:
            for k, v in b.writers.items():
                self._wait(eng, (k, v))
        for b in w:
            for k, v in b.writers.items():
                if accum and not isinstance(k, str) and not b.readers:
                    continue
                if k != eng or eng != "pe":
                    self._wait(eng, (k, v))
            for k, v in b.readers.items():
                if k != eng or eng != "pe":
                    self._wait(eng, (k, v))

    def _record(self, tok, r, w, accum=False):
        k, v = tok
        for b in r:
            b.readers[k] = v
        for b in w:
            if accum and not b.readers:
                b.writers[k] = v
            else:
                b.writers = {k: v}
            b.readers = {}

    @staticmethod
    def _bufs(xs):
        out = []
        for x in xs:
            if isinstance(x, Tile):
                out.append(x.b)
            elif isinstance(x, Buf):
                out.append(x)
            elif x is None:
                pass
            else:
                raise TypeError(x)
        return out

    def op(self, eng, r, w, emit):
        r = self._bufs(r)
        w = self._bufs(w)
        for b in r:
            if b.excl and b not in w:
                w = w + [b]
        self._deps(eng, r, w)
        ins = emit(self.E[eng])
        self.cnt[eng] += 1
        ins.then_inc(self.sem[eng], 1)
        self.ninst += 1
        self._record((eng, self.cnt[eng]), r, w)

    def dma(self, q, out, in_, r, w, **kw):
        r = self._bufs(r)
        w = self._bufs(w)
        self._deps(q, r, w, accum=True)
        if q == "pool":
            i = self.dnext_pool
            self.dnext_pool = ND_SP + (i + 1 - ND_SP) % (ND - ND_SP)
        else:
            i = self.dnext
            self.dnext = (i + 1) % ND_SP
        if self.dcnt[i] > 0:
            self._wait(q, (("d", i), self.dcnt[i]))
        self.dcnt[i] += 16
        ins = self.E[q].dma_start(out=out, in_=in_, **kw)
        ins.then_inc(self.dsem[i], 16)
        self.ninst += 1
        self._record((("d", i), self.dcnt[i]), r, w, accum=True)

    def barrier(self, engines=None):
        for e in (engines or self.E):
            for k in self.E:
                if k != e and self.cnt[k] > 0:
                    self._wait(e, (k, self.cnt[k]))
            for i in range(ND):
                if self.dcnt[i] > 0:
                    self._wait(e, (("d", i), self.dcnt[i]))

    def finish(self):
        self.barrier(engines=["sp"])


class View:
    def __init__(self, ap, name="", buf=None):
        self.t = ap
        self.b = buf if buf is not None else Buf(name)


def _bufs_ext(xs):
    out = []
    for x in xs:
        if isinstance(x, (Tile, View)):
            out.append(x.b)
        elif isinstance(x, Buf):
            out.append(x)
        elif x is None:
            pass
        else:
            raise TypeError(x)
    return out


KB._bufs = staticmethod(_bufs_ext)

import numpy as np


def host_consts(T=2048):
    c = {}
    m = np.arange(128)
    d = m % 64
    P = np.zeros((128, 128), np.float32)
    part = np.where(d < 32, m + 32, m - 32)
    P[part, m] = 1.0
    c["c_rot_ret"] = P
    P2 = np.zeros((128, 128), np.float32)
    for mm in range(128):
        dd = mm % 64
        if dd < 8:
            P2[mm + 8, mm] = 1.0
        elif dd < 16:
            P2[mm - 8, mm] = 1.0
    c["c_rot_dsa"] = P2
    t = np.arange(T, dtype=np.float64)
    inv = 10000.0 ** (-np.arange(0, 64, 2, dtype=np.float64) / 64)
    ang = t[None, :] * inv[d % 32][:, None]
    sgn = np.where(d < 32, -1.0, 1.0)[:, None]
    c["c_cs_ret"] = np.stack([np.cos(ang), np.sin(ang) * sgn]).astype(np.float32)
    inv2 = 500000.0 ** (-np.arange(0, 16, 2, dtype=np.float64) / 16)
    cos2 = np.ones((128, T)); sin2 = np.zeros((128, T))
    for mm in range(128):
        dd = mm % 64
        if dd < 16:
            a = t * inv2[dd % 8]
            cos2[mm] = np.cos(a)
            sin2[mm] = np.sin(a) * (-1.0 if dd < 8 else 1.0)
    c["c_cs_dsa"] = np.stack([cos2, sin2]).astype(np.float32)
    gam = 1.0 - 2.0 ** (-5.0 - np.arange(8, dtype=np.float64))
    j = np.arange(128)[:, None]; i = np.arange(128)[None, :]
    maskT = np.zeros((128, 8, 128), np.float64)
    for h in range(8):
        maskT[:, h, :] = np.where(i >= j, gam[h] ** np.maximum(i - j, 0), 0.0) / 8.0
    c["c_ret_maskT"] = maskT.astype(np.float32)
    qdec = np.zeros((128, 4, 128), np.float64)
    for p in range(4):
        for mm in range(128):
            h = 2 * p + mm // 64
            qdec[mm, p, :] = gam[h] ** (np.arange(128) + 1.0)
    c["c_ret_qdec"] = qdec.astype(np.float32)
    kdec = np.zeros((128, 8), np.float64)
    for h in range(8):
        kdec[:, h] = gam[h] ** (127.0 - np.arange(128)) / 8.0
    c["c_ret_kdec"] = kdec.astype(np.float32)
    c["ret_cd"] = [float(g ** 128) for g in gam]
    c["c_ident"] = np.eye(128, dtype=np.float32)
    c["c_negmask"] = np.where(i <= j, 0.0, -1e30).astype(np.float32)
    tau = np.arange(128)[:, None]; sig = np.arange(128)[None, :]
    same = (tau // 64) == (sig // 64)
    c["c_maskN"] = (same & (sig < tau)).astype(np.float32)
    c["c_maskNT"] = c["c_maskN"].T.copy()
    c["c_maskIT"] = (same & (sig <= tau)).astype(np.float32).T.copy()
    c["c_blockones"] = ((m[:, None] // 64) == (m[None, :] // 64)).astype(np.float32)
    return c


D = 1024
DFF = 2816
EPS = 1e-6


def make_consts(kb, es):
    c = {}
    c["ones_f"] = kb.sb(es, [128, 128], F32, "ones_f")
    kb.op("dve", [], [c["ones_f"]], lambda e: e.memset(c["ones_f"].t[:], 1.0))
    return c


def rms_hT(kb, xt, gcol, hT, sqs, ps_ss, rs, consts, W, eps=EPS):
    ones = consts["ones_f"]
    for c in range(8):
        sq = sqs[c % 2]
        kb.op("act", [xt], [sq], lambda e: e.activation(out=sq.t[:, :W], in_=xt.t[:, c, :W], func=AF.Square))
        kb.op("pe", [sq, ones], [ps_ss],
              lambda e: e.matmul(ps_ss.t[:, :W], ones.t[:], sq.t[:, :W], start=(c == 0), stop=(c == 7)))
    kb.op("act", [ps_ss], [rs],
          lambda e: e.activation(out=rs.t[:, :W], in_=ps_ss.t[:, :W], func=AF.Sqrt, scale=1.0 / D, bias=eps_ap(kb)))
    kb.op("dve", [rs], [rs], lambda e: e.reciprocal(out=rs.t[:, :W], in_=rs.t[:, :W]))
    for c in range(8):
        kb.op("dve", [xt, rs, gcol], [hT],
              lambda e: e.scalar_tensor_tensor(out=hT.t[:, c, :W], in0=xt.t[:, c, :W], scalar=gcol.t[:, c:c + 1],
                                               in1=rs.t[:, :W], op0=ALU.mult, op1=ALU.mult))


_eps = {}


def eps_ap(kb):
    return _eps[id(kb)].t[:, 0:1]


def init_eps(kb, es):
    t = kb.sb(es, [128, 1], F32, "epsc")
    kb.op("dve", [], [t], lambda e: e.memset(t.t[:], EPS))
    _eps[id(kb)] = t


def load_w_bf16(kb, dst, src_d, nchunk, ncols, split=2):
    step = ncols // split
    for c in range(nchunk):
        for s in range(split):
            kb.dma("pool", dst.t[:, c, s * step:(s + 1) * step], src_d[c * 128:(c + 1) * 128, s * step:(s + 1) * step],
                   r=[], w=[dst])


def stage_ffn(kb, xT_d, xbufs, NT, w_in_d, w_out_d, g_d, consts, TT=512):
    nc = kb.nc
    with contextlib.ExitStack() as es:
        w1 = kb.sb(es, [128, 8, 2 * DFF], BF16, "w1")
        w2 = kb.sb(es, [128, 22, D], BF16, "w2")
        gcol = kb.sb(es, [128, 8], F32, "gcol")
        xts = [kb.sb(es, [128, 8, TT], F32, "xt") for _ in range(2)]
        sqs = [kb.sb(es, [128, TT], F32, "sq") for _ in range(2)]
        rs = kb.sb(es, [128, TT], F32, "rs")
        hT = kb.sb(es, [128, 8, TT], BF16, "hT")
        aT = kb.sb(es, [128, 22, TT], BF16, "aT")
        sgs = [kb.sb(es, [128, TT], BF16, "sg") for _ in range(2)]
        ps_ss = kb.ps(es, [128, TT], F32, "ps_ss")
        psg = [kb.ps(es, [128, TT], F32, "psg") for _ in range(2)]
        psu = [kb.ps(es, [128, TT], F32, "psu") for _ in range(2)]
        pso = [kb.ps(es, [128, TT], F32, "pso") for _ in range(2)]

        kb.dma("sp", gcol.t[:, :], g_d.rearrange("(c p) -> p c", p=128), r=[], w=[gcol],
               allow_slow_non_contiguous=True)
        load_w_bf16(kb, w1, w_in_d, 8, 2 * DFF)
        load_w_bf16(kb, w2, w_out_d, 22, D)

        for ti in range(NT // TT):
            xt = xts[ti % 2]
            sl = slice(ti * TT, (ti + 1) * TT)
            kb.dma("sp", xt.t[:, :, :], xT_d[:, :, sl], r=[xbufs[ti]], w=[xt])
            rms_hT(kb, xt, gcol, hT, sqs, ps_ss, rs, consts, TT)
            for fb in range(22):
                pg, pu, sg = psg[fb % 2], psu[fb % 2], sgs[fb % 2]
                kb.op("pe", [w1, hT], [pg],
                      lambda e: [e.matmul(pg.t[:], w1.t[:, c, fb * 128:(fb + 1) * 128], hT.t[:, c, :],
                                          start=(c == 0), stop=(c == 7)) for c in range(8)][-1])
                kb.op("pe", [w1, hT], [pu],
                      lambda e: [e.matmul(pu.t[:], w1.t[:, c, DFF + fb * 128:DFF + (fb + 1) * 128], hT.t[:, c, :],
                                          start=(c == 0), stop=(c == 7)) for c in range(8)][-1])
                kb.op("act", [pg], [sg], lambda e: e.activation(out=sg.t[:], in_=pg.t[:], func=AF.Silu))
                kb.op("dve", [sg, pu], [aT],
                      lambda e: e.tensor_tensor(out=aT.t[:, fb, :], in0=sg.t[:], in1=pu.t[:], op=ALU.mult))
            for db in range(8):
                po = pso[db % 2]
                kb.op("pe", [w2, aT], [po],
                      lambda e: [e.matmul(po.t[:], w2.t[:, fc, db * 128:(db + 1) * 128], aT.t[:, fc, :],
                                          start=(fc == 0), stop=(fc == 21)) for fc in range(22)][-1])
                kb.op("dve", [po, xt], [xt],
                      lambda e: e.scalar_tensor_tensor(out=xt.t[:, db, :], in0=po.t[:], scalar=0.5, in1=xt.t[:, db, :],
                                                       op0=ALU.mult, op1=ALU.add))
            kb.dma("sp", xT_d[:, :, sl], xt.t[:, :, :], r=[xt], w=[xbufs[ti]])
    kb.barrier()

import os
STOPAT = int(os.environ.get("STOPAT", "99"))

OFF_RET = 0


def load_cols_bf16(kb, dst, w_d, col0, ncols):
    for c in range(8):
        kb.dma("pool", dst.t[:, c, :], w_d[c * 128:(c + 1) * 128, col0:col0 + ncols], r=[], w=[dst])


def proj_fm(kb, ps, w, wcol0, M, hT, tok0, W, out_p0=0):
    kb.op("pe", [w, hT], [ps],
          lambda e: [e.matmul(ps.t[out_p0:out_p0 + M, :W], w.t[:, c, wcol0:wcol0 + M], hT.t[:, c, tok0:tok0 + W],
                              start=(c == 0), stop=(c == 7)) for c in range(8)][-1])


def rope_fm(kb, ps, raw, psr, rot, cs, tok0, W, t1, t2, dst, dcol0, P=128):
    kb.op("act", [ps], [raw], lambda e: e.activation(out=raw.t[:P, :W], in_=ps.t[:P, :W], func=AF.Copy))
    kb.op("pe", [rot, raw], [psr], lambda e: e.matmul(psr.t[:P, :W], rot.t[:P, :P], raw.t[:P, :W], start=True, stop=True))
    kb.op("dve", [raw, cs], [t1],
          lambda e: e.tensor_tensor(out=t1.t[:P, :W], in0=raw.t[:P, :W], in1=cs.t[:P, 0, tok0:tok0 + W], op=ALU.mult))
    kb.op("dve", [psr, cs], [t2],
          lambda e: e.tensor_tensor(out=t2.t[:P, :W], in0=psr.t[:P, :W], in1=cs.t[:P, 1, tok0:tok0 + W], op=ALU.mult))
    kb.op("pool", [t1, t2], [dst],
          lambda e: e.tensor_tensor(out=dst.t[:P, dcol0:dcol0 + W], in0=t1.t[:P, :W], in1=t2.t[:P, :W], op=ALU.add))


def stage_retention(kb, hT, T, seq_off, w_d, yT_d, ybuf, C, G):
    nc = kb.nc
    NCH = T // 128
    NTT = T // 512
    with contextlib.ExitStack() as es:
        wq = kb.sb(es, [128, 8, 512], BF16, "wq")
        wk = kb.sb(es, [128, 8, 512], BF16, "wk")
        wv = kb.sb(es, [128, 8, 1024], BF16, "wv")
        wg = kb.sb(es, [128, 8, 1024], BF16, "wg")
        load_cols_bf16(kb, wv, w_d, OFF_RET + 1024, 1024)
        load_cols_bf16(kb, wq, w_d, OFF_RET + 0, 512)
        load_cols_bf16(kb, wk, w_d, OFF_RET + 512, 512)
        load_cols_bf16(kb, wg, w_d, OFF_RET + 2048, 1024)
        cs = kb.sb(es, [128, 2, T], F32, "cs")
        kb.dma("sp", cs.t[:, :, :], C["c_cs_ret"].rearrange("a p t -> p a t")[:, :, 0:T], r=[], w=[cs])
        rot = kb.sb(es, [128, 128], BF16, "rot")
        kb.dma("pool", rot.t[:, :], C["c_rot_ret"], r=[], w=[rot])
        maskT = kb.sb(es, [128, 8, 128], F32, "maskT")
        kb.dma("sp", maskT.t[:, :, :], C["c_ret_maskT"], r=[], w=[maskT])
        qdec = kb.sb(es, [128, 4, 128], F32, "qdec")
        kb.dma("sp", qdec.t[:, :, :], C["c_ret_qdec"], r=[], w=[qdec])
        kdec = kb.sb(es, [128, 8], F32, "kdec")
        kb.dma("sp", kdec.t[:, :], C["c_ret_kdec"], r=[], w=[kdec])
        v_sb = kb.sb(es, [128, NCH, 1024], BF16, "v_sb")
        qr = kb.sb(es, [128, T], BF16, "qr")
        kr = kb.sb(es, [128, T], BF16, "kr")
        qd = kb.sb(es, [128, T], BF16, "qd")
        sg = kb.sb(es, [128, T], BF16, "sg")
        o_sb = kb.sb(es, [128, T], F32, "o_sb")
        raw = kb.sb(es, [128, 512], BF16, "raw")
        t1 = kb.sb(es, [128, 512], F32, "t1")
        t2 = kb.sb(es, [128, 512], F32, "t2")
        sTm = [kb.sb(es, [128, 128], BF16, "sTm") for _ in range(2)]
        kd = kb.sb(es, [128, 64], BF16, "kd")
        state = kb.sb(es, [128, 128], F32, "state")
        state_bf = kb.sb(es, [128, 128], BF16, "state_bf")
        sq = kb.sb(es, [128, 512], F32, "sq")
        rs = kb.sb(es, [128, 512], F32, "rs")
        yo = [kb.sb(es, [128, 512], BF16, "yo") for _ in range(2)]
        pproj = [kb.ps(es, [128, 512], F32, "pproj") for _ in range(2)]
        psr = kb.ps(es, [128, 512], F32, "psr")
        bankS = kb.ps(es, [128, 512], F32, "bankS")
        psS = [View(bankS.t[:, k * 128:(k + 1) * 128], f"psS{k}", buf=bankS.b) for k in range(2)]
        psO = [kb.ps(es, [128, 512], F32, "psO") for _ in range(2)]
        psT = kb.ps(es, [128, 1024], BF16, "psT")
        psU = kb.ps(es, [128, 512], F32, "psU")
        ident = G["ident_bf"]
        ones_f = G["ones_f"]
        np_ = [0]

        def nextp():
            np_[0] += 1
            return pproj[np_[0] % 2]

        for tb in range(NCH):
            for half in range(2):
                ps = nextp()
                kb.op("pe", [wv, hT], [ps],
                      lambda e: [e.matmul(ps.t[:, :], hT.t[:, c, tb * 128:(tb + 1) * 128],
                                          wv.t[:, c, half * 512:(half + 1) * 512], start=(c == 0), stop=(c == 7))
                                 for c in range(8)][-1])
                kb.op("act", [ps], [v_sb],
                      lambda e: e.activation(out=v_sb.t[:, tb, half * 512:(half + 1) * 512], in_=ps.t[:, :], func=AF.Copy))

        if STOPAT <= 1:
            kb.barrier(); return
        for hp in range(4):
            for (w, dst) in ((wq, qr), (wk, kr)):
                for tt in range(NTT):
                    ps = nextp()
                    proj_fm(kb, ps, w, hp * 128, 128, hT, tt * 512, 512)
                    rope_fm(kb, ps, raw, psr, rot, cs, tt * 512, 512, t1, t2, dst, tt * 512)
            if STOPAT <= 2:
                kb.barrier(); return
            kb.op("pool", [qr, qdec], [qd],
                  lambda e: e.tensor_tensor(out=qd.t[:, :].rearrange("p (c i) -> p c i", i=128),
                                            in0=qr.t[:, :].rearrange("p (c i) -> p c i", i=128),
                                            in1=qdec.t[:, hp:hp + 1, :].to_broadcast([128, NCH, 128]), op=ALU.mult))
            if STOPAT <= 3:
                kb.barrier(); return
            for hh in range(2):
                h = 2 * hp + hh
                pb = 64 * hh
                for tt in range(NTT):
                    ps = nextp()
                    proj_fm(kb, ps, wg, h * 128, 128, hT, tt * 512, 512)
                    kb.op("act", [ps], [sg],
                          lambda e: e.activation(out=sg.t[:, tt * 512:(tt + 1) * 512], in_=ps.t[:, :], func=AF.Silu))
                for i in range(NCH):
                    cols = slice(i * 128, (i + 1) * 128)
                    pS, pO, sm = psS[i % 2], psO[i % 2], sTm[i % 2]
                    kb.op("pe", [kr, qr], [pS],
                          lambda e: e.matmul(pS.t[:, :], kr.t[pb:pb + 64, cols], qr.t[pb:pb + 64, cols], start=True, stop=True))
                    kb.op("dve", [pS, maskT], [sm],
                          lambda e: e.tensor_tensor(out=sm.t[:, :], in0=pS.t[:, :], in1=maskT.t[:, h, :], op=ALU.mult))
                    kb.op("pe", [v_sb, sm], [pO],
                          lambda e: e.matmul(pO.t[:, 0:128], v_sb.t[:, i, h * 128:(h + 1) * 128], sm.t[:, :],
                                             start=True, stop=(i == 0)))
                    if i > 0:
                        kb.op("pe", [state_bf, qd], [pO],
                              lambda e: e.matmul(pO.t[:, 0:128], state_bf.t[pb:pb + 64, :], qd.t[pb:pb + 64, cols],
                                                 start=False, stop=True))
                    kb.op("act", [pO], [o_sb], lambda e: e.activation(out=o_sb.t[:, cols], in_=pO.t[:, 0:128], func=AF.Copy))
                    if STOPAT <= 4:
                        kb.barrier(); return
                    if i < NCH - 1:
                        kb.op("pe", [kr, ident], [psT],
                              lambda e: e.transpose(psT.t[:, 0:64], kr.t[pb:pb + 64, cols], ident.t[pb:pb + 64, pb:pb + 64]))
                        kb.op("act", [psT, kdec], [kd],
                              lambda e: e.activation(out=kd.t[:, :], in_=psT.t[:, 0:64], func=AF.Copy, scale=kdec.t[:, h:h + 1]))
                        kb.op("pe", [kd, v_sb], [psU],
                              lambda e: e.matmul(psU.t[pb:pb + 64, 0:128], kd.t[:, :], v_sb.t[:, i, h * 128:(h + 1) * 128],
                                                 start=True, stop=True))
                        if i == 0:
                            kb.op("dve", [psU], [state],
                                  lambda e: e.tensor_copy(out=state.t[pb:pb + 64, :], in_=psU.t[pb:pb + 64, 0:128]))
                        else:
                            kb.op("dve", [psU, state], [state],
                                  lambda e: e.scalar_tensor_tensor(out=state.t[pb:pb + 64, :], in0=state.t[pb:pb + 64, :],
                                                                   scalar=C["ret_cd"][h], in1=psU.t[pb:pb + 64, 0:128],
                                                                   op0=ALU.mult, op1=ALU.add))
                        kb.op("act", [state], [state_bf],
                              lambda e: e.activation(out=state_bf.t[pb:pb + 64, :], in_=state.t[pb:pb + 64, :], func=AF.Copy))
                if STOPAT <= 5:
                    kb.barrier(); return
                for tt in range(NTT):
                    tc_ = slice(tt * 512, (tt + 1) * 512)
                    ps = nextp()
                    y = yo[tt % 2]
                    kb.op("act", [o_sb], [sq], lambda e: e.activation(out=sq.t[:, :], in_=o_sb.t[:, tc_], func=AF.Square))
                    kb.op("pe", [sq, ones_f], [ps], lambda e: e.matmul(ps.t[:, :], ones_f.t[:, :], sq.t[:, :], start=True, stop=True))
                    kb.op("act", [ps], [rs],
                          lambda e: e.activation(out=rs.t[:, :], in_=ps.t[:, :], func=AF.Sqrt, scale=1.0 / 128, bias=eps_ap(kb)))
                    kb.op("dve", [rs], [rs], lambda e: e.reciprocal(out=rs.t[:, :], in_=rs.t[:, :]))
                    kb.op("dve", [rs, o_sb], [rs],
                          lambda e: e.tensor_tensor(out=rs.t[:, :], in0=rs.t[:, :], in1=o_sb.t[:, tc_], op=ALU.mult))
                    kb.op("dve", [rs, sg], [y],
                          lambda e: e.tensor_tensor(out=y.t[:, :], in0=rs.t[:, :], in1=sg.t[:, tc_], op=ALU.mult))
                    kb.dma("sp", yT_d[h * 128:(h + 1) * 128, seq_off + tt * 512: seq_off + (tt + 1) * 512], y.t[:, :],
                           r=[y], w=[ybuf])
    kb.barrier()


import os


def run_lockstep(gens):
    gens = list(gens)
    while gens:
        nxt = []
        for g in gens:
            try:
                next(g)
                nxt.append(g)
            except StopIteration:
                pass
        gens = nxt

OFF_DSA = 3072
NEG_FILL = -3.0e38
NIT = 22
SGN_SCALE = float(2 ** 20)
EXACT_QB = 1


def rope_fm2(kb, ps, raw, psr, rot, cs, tok0, W, t1, t2, dst, dst_ap):
    kb.op("act", [ps], [raw], lambda e: e.activation(out=raw.t[:, :W], in_=ps.t[:, :W], func=AF.Copy))
    kb.op("pe", [rot, raw], [psr], lambda e: e.matmul(psr.t[:, :W], rot.t[:, :], raw.t[:, :W], start=True, stop=True))
    kb.op("dve", [raw, cs], [t1],
          lambda e: e.tensor_tensor(out=t1.t[:, :W], in0=raw.t[:, :W], in1=cs.t[:, 0, tok0:tok0 + W], op=ALU.mult))
    kb.op("dve", [psr, cs], [t2],
          lambda e: e.tensor_tensor(out=t2.t[:, :W], in0=psr.t[:, :W], in1=cs.t[:, 1, tok0:tok0 + W], op=ALU.mult))
    kb.op("pool", [t1, t2], [dst],
          lambda e: e.tensor_tensor(out=dst_ap, in0=t1.t[:, :W], in1=t2.t[:, :W], op=ALU.add))


def stage_dsa(kb, hT, T, seq_off, w_d, yT_d, ybuf, C, G, ktop=256):
    NQB = T // 128
    NTT = T // 512
    ident = G["ident_bf"]
    with contextlib.ExitStack() as es:
        negmask = kb.sb(es, [128, 128], F32, "negmask")
        kb.dma("sp", negmask.t[:, :], C["c_negmask"], r=[], w=[negmask])
        qr = kb.sb(es, [128, 4, T], BF16, "qr")
        qir = kb.sb(es, [128, 2, T], BF16, "qir")
        kr2 = kb.sb(es, [128, T], BF16, "kr2")
        kir2 = kb.sb(es, [128, T], BF16, "kir2")
        v_sb = kb.sb(es, [128, NQB, 64], BF16, "v_sb")
        ones64 = kb.sb(es, [128, 64], BF16, "ones64")
        kb.op("dve", [], [ones64], lambda e: e.memset(ones64.t[:, :], 1.0))
        wabs = kb.sb(es, [128, NQB, 4], F32, "wabs")
        wsgn = kb.sb(es, [128, NQB, 4], F32, "wsgn")

        with contextlib.ExitStack() as ea:
            wd = kb.sb(ea, [128, 8, 964], BF16, "wd")
            load_cols_bf16(kb, wd, w_d, OFF_DSA, 964)
            cs = kb.sb(ea, [128, 2, T], F32, "cs")
            kb.dma("sp", cs.t[:, :, :], C["c_cs_dsa"].rearrange("a p t -> p a t")[:, :, 0:T], r=[], w=[cs])
            rot = kb.sb(ea, [128, 128], BF16, "rot")
            kb.dma("pool", rot.t[:, :], C["c_rot_dsa"], r=[], w=[rot])
            pproj = [kb.ps(ea, [128, 512], F32, "pproj") for _ in range(2)]
            psr = kb.ps(ea, [128, 512], F32, "psr")
            raw = kb.sb(ea, [128, 512], BF16, "raw")
            t1 = kb.sb(ea, [128, 512], F32, "t1")
            t2 = kb.sb(ea, [128, 512], F32, "t2")
            n_ = [0]

            def nextp():
                n_[0] += 1
                return pproj[n_[0] % 2]

            for tt in range(NTT):
                tok0 = tt * 512
                for j in range(4):
                    ps = nextp()
                    proj_fm(kb, ps, wd, j * 128, 128, hT, tok0, 512)
                    rope_fm2(kb, ps, raw, psr, rot, cs, tok0, 512, t1, t2, qr, qr.t[:, j, tok0:tok0 + 512])
                for j in range(2):
                    ps = nextp()
                    proj_fm(kb, ps, wd, 640 + j * 128, 128, hT, tok0, 512)
                    rope_fm2(kb, ps, raw, psr, rot, cs, tok0, 512, t1, t2, qir, qir.t[:, j, tok0:tok0 + 512])
                for (col, dst) in ((512, kr2), (896, kir2)):
                    ps = nextp()
                    proj_fm(kb, ps, wd, col, 64, hT, tok0, 512, out_p0=0)
                    proj_fm(kb, ps, wd, col, 64, hT, tok0, 512, out_p0=64)
                    rope_fm2(kb, ps, raw, psr, rot, cs, tok0, 512, t1, t2, dst, dst.t[:, tok0:tok0 + 512])
            for qb in range(NQB):
                ps = nextp()
                qc = slice(qb * 128, (qb + 1) * 128)
                for c in range(8):
                    kb.op("pe", [wd, hT], [ps],
                          lambda e: e.matmul(ps.t[:, 0:64], hT.t[:, c, qc], wd.t[:, c, 576:640], start=(c == 0), stop=(c == 7)))
                for c in range(8):
                    kb.op("pe", [wd, hT], [ps],
                          lambda e: e.matmul(ps.t[:, 64:68], hT.t[:, c, qc], wd.t[:, c, 960:964], start=(c == 0), stop=(c == 7)))
                kb.op("act", [ps], [v_sb], lambda e: e.activation(out=v_sb.t[:, qb, :], in_=ps.t[:, 0:64], func=AF.Copy))
                kb.op("act", [ps], [wabs],
                      lambda e: e.activation(out=wabs.t[:, qb, :], in_=ps.t[:, 64:68], func=AF.Abs, scale=0.5 / 8.0))
                kb.op("act", [ps], [wsgn], lambda e: e.activation(out=wsgn.t[:, qb, :], in_=ps.t[:, 64:68], func=AF.Sign))
        kb.barrier()

        with contextlib.ExitStack() as eb:
            NC3 = 3
            psI = [kb.ps(eb, [128, 512], F32, "psI") for _ in range(2)]
            psMT = kb.ps(eb, [128, 1024], BF16, "psMT")
            psS = [kb.ps(eb, [128, 512], F32, "psS") for _ in range(2)]
            psO = kb.ps(eb, [128, 512], F32, "psO")
            psD = kb.ps(eb, [128, 512], F32, "psD")
            scr = kb.sb(eb, [128, T], BF16, "scr")
            onesT = kb.sb(eb, [128, T], BF16, "onesT")
            kb.op("dve", [], [onesT], lambda e: e.memset(onesT.t[:, :], 1.0))
            pw2 = kb.sb(eb, [128, NIT + 1], F32, "pw2")
            for k in range(NIT + 1):
                kb.op("dve", [], [pw2], lambda e: e.memset(pw2.t[:, k:k + 1], 2.0 ** (-k)))
            pT = [kb.sb(eb, [128, 512], BF16, "pT") for _ in range(2)]
            rD = kb.sb(eb, [128, 512], F32, "rD")
            yb = [kb.sb(eb, [128, 512], BF16, "yb") for _ in range(2)]
            ctxs = []
            for g in range(NC3):
                cx = {}
                cx["sc"] = kb.sb(eb, [128, T], F32, "sc")
                cx["junk"] = kb.sb(eb, [128, T], BF16, "junk")
                cx["csum"] = kb.sb(eb, [128, T], F32, "csum")
                cx["mask"] = kb.sb(eb, [128, T], BF16, "mask")
                cx["maskT"] = kb.sb(eb, [128, NQB, 128], BF16, "maskT")
                cx["rl"] = [kb.sb(eb, [128, 512], F32, "rl") for _ in range(2)]
                for nm, w_ in (("cnt", 1), ("ge", 1), ("lo", 1), ("negmid", 1), ("amax", 1), ("gpos", 1), ("halfs", NIT + 1)):
                    cx[nm] = kb.sb(eb, [128, w_], F32, nm)
                ctxs.append(cx)
            itc = [0]

            def sel_gen(qb, cx):
                sc, junk, csum, mask, maskT = cx["sc"], cx["junk"], cx["csum"], cx["mask"], cx["maskT"]
                cnt, ge, lo, negmid, amax, gpos, halfs = (cx[n] for n in ("cnt", "ge", "lo", "negmid", "amax", "gpos", "halfs"))
                nk = 128 * (qb + 1)
                qc = slice(qb * 128, (qb + 1) * 128)
                for kc in range((nk + 511) // 512):
                    kw = min(512, nk - kc * 512)
                    kcols = slice(kc * 512, kc * 512 + kw)
                    for h4 in range(4):
                        pair, pb = h4 // 2, 64 * (h4 % 2)
                        itc[0] += 1
                        pI, r_ = psI[itc[0] % 2], cx["rl"][h4 % 2]
                        kb.op("pe", [qir, kir2], [pI],
                              lambda e: e.matmul(pI.t[:, :kw], qir.t[pb:pb + 64, pair, qc], kir2.t[pb:pb + 64, kcols],
                                                 start=True, stop=True))
                        kb.op("act", [pI, wabs], [r_],
                              lambda e: e.activation(out=r_.t[:, :kw], in_=pI.t[:, :kw], func=AF.Relu,
                                                     scale=wabs.t[:, qb, h4:h4 + 1]))
                        if h4 == 0:
                            kb.op("dve", [r_, wsgn], [sc],
                                  lambda e: e.tensor_scalar(out=sc.t[:, kcols], in0=r_.t[:, :kw], scalar1=wsgn.t[:, qb, 0:1],
                                                            scalar2=None, op0=ALU.mult))
                        else:
                            kb.op("dve", [r_, wsgn, sc], [sc],
                                  lambda e: e.scalar_tensor_tensor(out=sc.t[:, kcols], in0=r_.t[:, :kw],
                                                                   scalar=wsgn.t[:, qb, h4:h4 + 1], in1=sc.t[:, kcols],
                                                                   op0=ALU.mult, op1=ALU.add))
                        yield
                if nk > ktop:
                    kb.op("dve", [sc], [amax],
                          lambda e: e.tensor_reduce(out=amax.t[:, :], in_=sc.t[:, :nk], axis=AX.X, op=ALU.max,
                                                    apply_absolute_value=True))
                    kb.op("dve", [amax], [amax],
                          lambda e: e.tensor_scalar(out=amax.t[:, :], in0=amax.t[:, :], scalar1=1.0, scalar2=None, op0=ALU.add))
                    kb.op("dve", [amax, pw2], [halfs],
                          lambda e: e.tensor_scalar(out=halfs.t[:, :], in0=pw2.t[:, :], scalar1=amax.t[:, 0:1], scalar2=1.25,
                                                    op0=ALU.mult, op1=ALU.mult))
                    kb.op("dve", [amax], [lo],
                          lambda e: e.tensor_scalar(out=lo.t[:, :], in0=amax.t[:, :], scalar1=-1.5, scalar2=None, op0=ALU.mult))
                    kb.op("dve", [lo, halfs], [negmid],
                          lambda e: e.tensor_scalar(out=negmid.t[:, :], in0=lo.t[:, :], scalar1=halfs.t[:, 0:1], scalar2=-SGN_SCALE,
                                                    op0=ALU.add, op1=ALU.mult))
                kb.op("dve", [sc, negmask], [sc],
                      lambda e: e.tensor_tensor(out=sc.t[:, qc], in0=sc.t[:, qc], in1=negmask.t[:, :], op=ALU.add))
                yield
                if nk > ktop:
                    for k in range(NIT):
                        kb.op("act", [sc, negmid], [junk, cnt],
                              lambda e: e.activation(out=junk.t[:, :nk], in_=sc.t[:, :nk], func=AF.Sign, bias=negmid.t[:, 0:1],
                                                     scale=SGN_SCALE, accum_out=cnt.t[:, 0:1]))
                        yield
                        kb.op("dve", [cnt], [ge],
                              lambda e: e.tensor_scalar(out=ge.t[:, :], in0=cnt.t[:, :], scalar1=float(2 * ktop - nk), scalar2=None,
                                                        op0=ALU.is_ge))
                        kb.op("dve", [ge, halfs, lo], [lo],
                              lambda e: e.scalar_tensor_tensor(out=lo.t[:, :], in0=ge.t[:, :], scalar=halfs.t[:, k:k + 1],
                                                               in1=lo.t[:, :], op0=ALU.mult, op1=ALU.add))
                        kb.op("dve", [lo, halfs], [negmid],
                              lambda e: e.tensor_scalar(out=negmid.t[:, :], in0=lo.t[:, :], scalar1=halfs.t[:, k + 1:k + 2],
                                                        scalar2=-SGN_SCALE, op0=ALU.add, op1=ALU.mult))
                        yield
                    kb.op("dve", [sc, lo], [mask],
                          lambda e: e.tensor_scalar(out=mask.t[:, :nk], in0=sc.t[:, :nk], scalar1=lo.t[:, 0:1], scalar2=None,
                                                    op0=ALU.is_gt))
                    yield
                    kb.op("dve", [sc], [junk],
                          lambda e: e.tensor_scalar(out=junk.t[:, :nk], in0=sc.t[:, :nk], scalar1=0.0, scalar2=None, op0=ALU.is_equal))
                    yield
                    kb.op("dve", [sc], [scr, gpos],
                          lambda e: e.tensor_scalar(out=scr.t[:, :nk], in0=sc.t[:, :nk], scalar1=0.0, scalar2=None, op0=ALU.is_gt,
                                                    op1=ALU.add, accum_out=gpos.t[:, 0:1]))
                    kb.op("dve", [gpos], [gpos],
                          lambda e: e.tensor_scalar(out=gpos.t[:, :], in0=gpos.t[:, :], scalar1=-1.0, scalar2=float(ktop),
                                                    op0=ALU.mult, op1=ALU.add))
                    kb.op("dve", [gpos], [gpos],
                          lambda e: e.tensor_scalar(out=gpos.t[:, :], in0=gpos.t[:, :], scalar1=0.0, scalar2=None, op0=ALU.max))
                    yield
                    kb.op("dve", [junk, onesT], [csum],
                          lambda e: e.tensor_tensor_scan(out=csum.t[:, :nk], data0=onesT.t[:, :nk], data1=junk.t[:, :nk], initial=0.0,
                                                         op0=ALU.mult, op1=ALU.add))
                    yield
                    kb.op("dve", [junk, csum], [csum],
                          lambda e: e.tensor_tensor(out=csum.t[:, :nk], in0=csum.t[:, :nk], in1=junk.t[:, :nk], op=ALU.mult))
                    yield
                    kb.op("dve", [csum, gpos, mask], [mask],
                          lambda e: e.scalar_tensor_tensor(out=mask.t[:, :nk], in0=csum.t[:, :nk], scalar=gpos.t[:, 0:1],
                                                           in1=mask.t[:, :nk], op0=ALU.is_le, op1=ALU.mult))
                else:
                    kb.op("dve", [sc], [mask],
                          lambda e: e.tensor_scalar(out=mask.t[:, :nk], in0=sc.t[:, :nk], scalar1=-1.0e29, scalar2=None,
                                                    op0=ALU.is_gt))
                yield
                for kb0 in range(0, qb + 1, 8):
                    nb = min(8, qb + 1 - kb0)
                    for k2 in range(nb):
                        kbi = kb0 + k2
                        kb.op("pe", [mask, ident], [psMT],
                              lambda e: e.transpose(psMT.t[:, k2 * 128:(k2 + 1) * 128], mask.t[:, kbi * 128:(kbi + 1) * 128],
                                                    ident.t[:, :]))
                    kb.op("act", [psMT], [maskT],
                          lambda e: e.activation(out=maskT.t[:, kb0:kb0 + nb, :].rearrange("p a b -> p (a b)"),
                                                 in_=psMT.t[:, 0:nb * 128], func=AF.Copy))
                    yield

            def attn(qb, cx):
                maskT = cx["maskT"]
                qc = slice(qb * 128, (qb + 1) * 128)
                for par in range(2):
                    pb = 64 * par
                    for kbi in range(qb + 1):
                        itc[0] += 1
                        pS, p_ = psS[itc[0] % 2], pT[itc[0] % 2]
                        kc_ = slice(kbi * 128, (kbi + 1) * 128)
                        kb.op("pe", [kr2, qr], [pS],
                              lambda e: e.matmul(pS.t[:, :].rearrange("p (a b) -> p a b", b=128), kr2.t[pb:pb + 64, kc_],
                                                 qr.t[pb:pb + 64, :, qc], start=True, stop=True))
                        kb.op("act", [pS], [p_], lambda e: e.activation(out=p_.t[:, :], in_=pS.t[:, :], func=AF.Exp, scale=0.125))
                        kb.op("dve", [p_, maskT], [p_],
                              lambda e: e.tensor_tensor(out=p_.t[:, :].rearrange("p (a b) -> p a b", b=128),
                                                        in0=p_.t[:, :].rearrange("p (a b) -> p a b", b=128),
                                                        in1=maskT.t[:, kbi:kbi + 1, :].to_broadcast([128, 4, 128]), op=ALU.mult))
                        kb.op("pe", [v_sb, p_], [psO],
                              lambda e: e.matmul(psO.t[0:64, :], v_sb.t[:, kbi, :], p_.t[:, :], start=(kbi == 0), stop=(kbi == qb)))
                        kb.op("pe", [ones64, p_], [psD],
                              lambda e: e.matmul(psD.t[0:64, :], ones64.t[:, :], p_.t[:, :], start=(kbi == 0), stop=(kbi == qb)))
                    y_ = yb[par]
                    kb.op("dve", [psD], [rD], lambda e: e.reciprocal(out=rD.t[0:64, :], in_=psD.t[0:64, :]))
                    kb.op("dve", [psO, rD], [y_],
                          lambda e: e.tensor_tensor(out=y_.t[0:64, :], in0=psO.t[0:64, :], in1=rD.t[0:64, :], op=ALU.mult))
                    dst = yT_d[1024:1536, seq_off + qb * 128: seq_off + (qb + 1) * 128].rearrange(
                        "(j two d) t -> two d j t", two=2, d=64)[par]
                    kb.dma("sp", dst, y_.t[0:64, :].rearrange("p (a b) -> p a b", b=128), r=[y_], w=[ybuf])

            for g0 in range(0, NQB, NC3):
                grp = list(range(g0, min(NQB, g0 + NC3)))
                run_lockstep([sel_gen(qb, ctxs[i]) for i, qb in enumerate(grp)])
                for i, qb in enumerate(grp):
                    attn(qb, ctxs[i])
    kb.barrier()

import math, os
STOPAT = int(os.environ.get('STOPAT', '99'))

OFF_RWKV = 4036
POOLENG = os.environ.get("POOLENG", "dve")
EM05 = math.exp(-0.5)
GN_EPS = 64e-5


def run_lockstep(gens):
    gens = list(gens)
    while gens:
        nxt = []
        for g in gens:
            try:
                next(g)
                nxt.append(g)
            except StopIteration:
                pass
        gens = nxt


def stage_rwkv(kb, hT, T, seq_off, w_d, PL, yT_d, ybuf, C, G):
    NTT = T // 512
    NB = T // 128
    NCK = T // 64
    ident_bf = G["ident_bf"]
    ident_f = G["ident_f"]
    with contextlib.ExitStack() as es:
        colp = kb.sb(es, [128, 4, 8], F32, "colp")
        for idx, nm in enumerate(["rwkv_w0", "rwkv_a0", "rwkv_k_k", "rwkv_k_a", "rwkv_r_k", "rwkv_ln_w", "rwkv_ln_b"]):
            kb.dma("sp", colp.t[:, :, idx], PL[nm].rearrange("(j p) -> p j", p=128), r=[], w=[colp],
                   allow_slow_non_contiguous=True)
        kb.op("dve", [colp], [colp],
              lambda e: e.tensor_scalar(out=colp.t[:, :, 7], in0=colp.t[:, :, 3], scalar1=-1.0, scalar2=1.0, op0=ALU.mult, op1=ALU.add))
        mu = kb.sb(es, [128, 14, 2], F32, "mu")
        kb.dma("sp", mu.t[:, :, 0], PL["rwkv_mu"].rearrange("(f p) -> p f", p=128), r=[], w=[mu], allow_slow_non_contiguous=True)
        kb.op("dve", [mu], [mu],
              lambda e: e.tensor_scalar(out=mu.t[:, :, 1], in0=mu.t[:, :, 0], scalar1=-1.0, scalar2=1.0, op0=ALU.mult, op1=ALU.add))
        w2a2 = kb.sb(es, [128, 512], BF16, "w2a2")
        kb.dma("pool", w2a2.t[0:64, :], PL["rwkv_w2"], r=[], w=[w2a2])
        kb.dma("pool", w2a2.t[64:128, :], PL["rwkv_a2"], r=[], w=[w2a2])
        g2b = kb.sb(es, [128, 512], BF16, "g2b")
        kb.dma("pool", g2b.t[:, :], PL["rwkv_g2"], r=[], w=[g2b])
        blk1 = kb.sb(es, [128, 128], F32, "blk1")
        kb.dma("sp", blk1.t[:, :], C["c_blockones"], r=[], w=[blk1])
        mN = kb.sb(es, [128, 128], F32, "mN")
        mNT = kb.sb(es, [128, 128], F32, "mNT")
        mIT = kb.sb(es, [128, 128], F32, "mIT")
        kb.dma("sp", mN.t[:, :], C["c_maskN"], r=[], w=[mN])
        kb.dma("sp", mNT.t[:, :], C["c_maskNT"], r=[], w=[mNT])
        kb.dma("sp", mIT.t[:, :], C["c_maskIT"], r=[], w=[mIT])
        resetm = kb.sb(es, [128, T], BF16, "resetm")
        kb.op("dve", [], [resetm], lambda e: e.memset(resetm.t[:, :], 1.0))
        kb.op("dve", [resetm], [resetm],
              lambda e: e.memset(resetm.t[:, :].rearrange("p (c i) -> p c i", i=64)[:, :, 0:1], 0.0))
        wl = kb.sb(es, [128, 8, 256], BF16, "wl")
        for c in range(8):
            kb.dma("pool", wl.t[:, c, :], w_d[c * 128:(c + 1) * 128, OFF_RWKV + 1536:OFF_RWKV + 1792], r=[], w=[wl])
        wp = [kb.sb(es, [128, 8, 384], BF16, "wp")]
        lo = kb.sb(es, [128, T], BF16, "lo")
        sgl = kb.sb(es, [128, T], BF16, "sgl")
        S = [kb.sb(es, [128, T + (1 if i == 7 else 0)], F32, f"S{i}") if i != 2 else None for i in range(8)]
        g_bf = kb.sb(es, [128, T], BF16, "g_bf")
        At = kb.sb(es, [128, T], BF16, "At")
        Bt = kb.sb(es, [128, T], BF16, "Bt")
        BtP = kb.sb(es, [128, T], BF16, "BtP")
        Kt = kb.sb(es, [128, T], BF16, "Kt")
        KtP = kb.sb(es, [128, T], BF16, "KtP")
        Rt = kb.sb(es, [128, T], BF16, "Rt")
        vb = kb.sb(es, [128, T], BF16, "vb")
        tok = kb.sb(es, [128, NB, 512], BF16, "tok")
        PCc = kb.sb(es, [128, NCK], F32, "PCc")
        ApT_all = kb.sb(es, [128, NB, 128], BF16, "ApT_all")
        W_all = kb.sb(es, [128, NB, 2, 64], F32, "W_all")
        ArbT_all = kb.sb(es, [128, NB, 2, 128], BF16, "ArbT_all")
        ArkT_all = kb.sb(es, [128, NB, 2, 128], BF16, "ArkT_all")
        Hf = kb.sb(es, [128, 64], F32, "Hf")
        Hb_all = kb.sb(es, [128, NCK + 1, 64], BF16, "Hb_all")
        rn = kb.sb(es, [128, 512], F32, "rn")
        yo = [kb.sb(es, [128, 512], BF16, "yo") for _ in range(2)]

        def shift_block(ps_list, fb, PA, PB, dst_fn):
            pass

        BK = [kb.ps(es, [128, 512], F32, "bk") for _ in range(7)]
        with contextlib.ExitStack() as ea:
            pproj = BK[0:2]
            psT = kb.ps(ea, [128, 1024], BF16, "psT")
            n_ = [0]

            def nextp():
                n_[0] += 1
                return pproj[n_[0] % 2]

            x_ = [0]

            nextx = nextp

            PA, PB = S[6], S[7]
            kb.op("dve", [], [PB], lambda e: e.memset(PB.t[:, 0:1], 0.0))

            def shifted(w, wcol0, fb, dst):
                kb.op("dve", [], [PB], lambda e: e.memset(PB.t[:, 0:1], 0.0))
                for tt in range(NTT):
                    ps = nextp()
                    tc_ = slice(tt * 512, (tt + 1) * 512)
                    proj_fm(kb, ps, w, wcol0, 128, hT, tt * 512, 512)
                    kb.op("act", [ps, mu], [PA],
                          lambda e: e.activation(out=PA.t[:, tc_], in_=ps.t[:, :], func=AF.Copy, scale=mu.t[:, fb, 1:2]))
                    kb.op("dve", [ps, mu], [PB],
                          lambda e: e.tensor_scalar(out=PB.t[:, 1 + tt * 512:1 + (tt + 1) * 512], in0=ps.t[:, :],
                                                    scalar1=mu.t[:, fb, 0:1], scalar2=None, op0=ALU.mult))
                kb.op(POOLENG, [PA, PB], [dst],
                      lambda e: e.tensor_tensor(out=dst.t[:, 0:T], in0=PA.t[:, 0:T], in1=PB.t[:, 0:T], op=ALU.add))

            if STOPAT <= 1:
                kb.barrier(); return
            shifted(wl, 0, 12, S[0])
            kb.op("act", [S[0]], [lo], lambda e: e.activation(out=lo.t[0:64, :], in_=S[0].t[0:64, 0:T], func=AF.Tanh))
            kb.op("act", [S[0]], [lo], lambda e: e.activation(out=lo.t[64:128, :], in_=S[0].t[64:128, 0:T], func=AF.Copy))
            shifted(wl, 128, 13, S[0])
            kb.op("act", [S[0]], [sgl], lambda e: e.activation(out=sgl.t[:, :], in_=S[0].t[:, 0:T], func=AF.Sigmoid))

            if os.environ.get("REPEAT"):
                shifted(wl, 128, 13, S[0])
            if STOPAT <= 2:
                kb.barrier(); return
            for j in range(4):
                pc = slice(j * 128, (j + 1) * 128)
                wpj = wp[0]
                for c in range(0 if os.environ.get("SKIPDMA") else 8):
                    kb.dma("pool", wpj.t[:, c, :].rearrange("p (k f) -> p k f", k=3),
                           w_d[c * 128:(c + 1) * 128, OFF_RWKV:OFF_RWKV + 1536].rearrange("p (k f) -> p k f", k=3)[:, :, j * 128:(j + 1) * 128],
                           r=[], w=[wpj])
                r_, k_, v_ = S[0], S[1], vb
                shifted(wpj, 0, j, r_)
                if os.environ.get("ONLYR"):
                    kb.barrier(); return
                shifted(wpj, 128, 4 + j, k_)
                if not os.environ.get("SKIPV"):
                    shifted(wpj, 256, 8 + j, vb)
                cp = lambda idx: colp.t[:, j, idx:idx + 1]
                lw, a_, cum = S[3], S[4], S[5]
                if os.environ.get("STOP25"):
                    kb.barrier(); return
                for tt in range(NTT):
                    tc_ = slice(tt * 512, (tt + 1) * 512)
                    ps = nextx()
                    kb.op("pe", [w2a2, lo], [ps],
                          lambda e: e.matmul(ps.t[:, :], w2a2.t[0:64, pc], lo.t[0:64, tc_], start=True, stop=True))
                    kb.op("act", [ps, colp], [lw],
                          lambda e: e.activation(out=lw.t[:, tc_], in_=ps.t[:, :], func=AF.Sigmoid, bias=cp(0)))
                    ps = nextx()
                    kb.op("pe", [w2a2, lo], [ps],
                          lambda e: e.matmul(ps.t[:, :], w2a2.t[64:128, pc], lo.t[64:128, tc_], start=True, stop=True))
                    kb.op("act", [ps, colp], [a_],
                          lambda e: e.activation(out=a_.t[:, tc_], in_=ps.t[:, :], func=AF.Sigmoid, bias=cp(1)))
                    ps = nextx()
                    kb.op("pe", [g2b, sgl], [ps],
                          lambda e: e.matmul(ps.t[:, :], g2b.t[:, pc], sgl.t[:, tc_], start=True, stop=True))
                    kb.op("act", [ps], [g_bf], lambda e: e.activation(out=g_bf.t[:, tc_], in_=ps.t[:, :], func=AF.Copy))
                kb.op("dve", [lw], [lw],
                      lambda e: e.tensor_scalar(out=lw.t[:, 0:T], in0=lw.t[:, 0:T], scalar1=-EM05, scalar2=None, op0=ALU.mult))
                if os.environ.get("SKIPSCAN"):
                    kb.barrier(); return
                kb.op("dve", [resetm, lw], [cum],
                      lambda e: e.tensor_tensor_scan(out=cum.t[:, 0:T], data0=resetm.t[:, :], data1=lw.t[:, 0:T], initial=0.0,
                                                     op0=ALU.mult, op1=ALU.add))
                if STOPAT <= 3:
                    kb.barrier(); return
                kb.op(POOLENG, [cum, lw], [S[6]],
                      lambda e: e.tensor_tensor(out=S[6].t[:, 0:T], in0=cum.t[:, 0:T], in1=lw.t[:, 0:T], op=ALU.subtract))
                kb.op("act", [S[6]], [S[6]], lambda e: e.activation(out=S[6].t[:, 0:T], in_=S[6].t[:, 0:T], func=AF.Exp))
                kk = S[7]
                for tt in range(NTT):
                    tc_ = slice(tt * 512, (tt + 1) * 512)
                    ps = nextx()
                    kb.op("act", [k_, colp], [kk],
                          lambda e: e.activation(out=kk.t[:, tc_], in_=k_.t[:, tc_], func=AF.Square, scale=cp(2)))
                    kb.op("pe", [blk1, kk], [ps], lambda e: e.matmul(ps.t[:, :], blk1.t[:, :], kk.t[:, tc_], start=True, stop=True))
                    kb.op("act", [ps], [rn], lambda e: e.activation(out=rn.t[:, :], in_=ps.t[:, :], func=AF.Sqrt))
                    kb.op("dve", [rn], [rn],
                          lambda e: e.tensor_scalar(out=rn.t[:, :], in0=rn.t[:, :], scalar1=1e-12, scalar2=None, op0=ALU.max))
                    kb.op("dve", [rn], [rn], lambda e: e.reciprocal(out=rn.t[:, :], in_=rn.t[:, :]))
                    kb.op("dve", [k_, colp, rn], [kk],
                          lambda e: e.scalar_tensor_tensor(out=kk.t[:, tc_], in0=k_.t[:, tc_], scalar=cp(2), in1=rn.t[:, :],
                                                           op0=ALU.mult, op1=ALU.mult))
                kb.op("dve", [kk, S[6]], [At],
                      lambda e: e.scalar_tensor_tensor(out=At.t[:, :], in0=kk.t[:, 0:T], scalar=-1.0, in1=S[6].t[:, 0:T],
                                                       op0=ALU.mult, op1=ALU.mult))
                kka = S[6]
                kb.op(POOLENG, [kk, a_], [kka],
                      lambda e: e.tensor_tensor(out=kka.t[:, 0:T], in0=kk.t[:, 0:T], in1=a_.t[:, 0:T], op=ALU.mult))
                kb.op("dve", [a_, colp], [a_],
                      lambda e: e.tensor_scalar(out=a_.t[:, 0:T], in0=a_.t[:, 0:T], scalar1=cp(3), scalar2=cp(7), op0=ALU.mult,
                                                op1=ALU.add))
                kh = a_
                kb.op(POOLENG, [k_, a_], [kh],
                      lambda e: e.tensor_tensor(out=kh.t[:, 0:T], in0=k_.t[:, 0:T], in1=a_.t[:, 0:T], op=ALU.mult))
                inv = S[1]
                kb.op("act", [cum], [inv], lambda e: e.activation(out=inv.t[:, 0:T], in_=cum.t[:, 0:T], func=AF.Exp, scale=-1.0))
                kb.op("dve", [kka, inv], [Bt],
                      lambda e: e.tensor_tensor(out=Bt.t[:, :], in0=kka.t[:, 0:T], in1=inv.t[:, 0:T], op=ALU.mult))
                kb.op("dve", [kh, inv], [Kt],
                      lambda e: e.tensor_tensor(out=Kt.t[:, :], in0=kh.t[:, 0:T], in1=inv.t[:, 0:T], op=ALU.mult))
                E = S[1]
                cum3 = cum.t[:, 0:T].rearrange("p (c i) -> p c i", i=64)
                kb.op("dve", [cum], [E],
                      lambda e: e.tensor_tensor(out=E.t[:, 0:T].rearrange("p (c i) -> p c i", i=64),
                                                in0=cum3[:, :, 63:64].to_broadcast([128, NCK, 64]), in1=cum3, op=ALU.subtract))
                kb.op("act", [E], [E], lambda e: e.activation(out=E.t[:, 0:T], in_=E.t[:, 0:T], func=AF.Exp))
                kb.op("dve", [kka, E], [BtP],
                      lambda e: e.tensor_tensor(out=BtP.t[:, :], in0=kka.t[:, 0:T], in1=E.t[:, 0:T], op=ALU.mult))
                kb.op("dve", [kh, E], [KtP],
                      lambda e: e.tensor_tensor(out=KtP.t[:, :], in0=kh.t[:, 0:T], in1=E.t[:, 0:T], op=ALU.mult))
                Pinc = S[3]
                kb.op("act", [cum], [Pinc], lambda e: e.activation(out=Pinc.t[:, 0:T], in_=cum.t[:, 0:T], func=AF.Exp))
                kb.op("dve", [r_, Pinc], [Rt],
                      lambda e: e.tensor_tensor(out=Rt.t[:, :], in0=r_.t[:, 0:T], in1=Pinc.t[:, 0:T], op=ALU.mult))
                kb.op("dve", [Pinc], [PCc],
                      lambda e: e.tensor_copy(out=PCc.t[:, :], in_=Pinc.t[:, 0:T].rearrange("p (c i) -> p c i", i=64)[:, :, 63]))
                rk = S[1]
                kb.op("dve", [r_, colp, kh], [rk],
                      lambda e: e.scalar_tensor_tensor(out=rk.t[:, 0:T], in0=r_.t[:, 0:T], scalar=cp(4), in1=kh.t[:, 0:T],
                                                       op0=ALU.mult, op1=ALU.mult))
                bon = S[6]
                for tt in range(NTT):
                    tc_ = slice(tt * 512, (tt + 1) * 512)
                    ps = nextx()
                    kb.op("pe", [blk1, rk], [ps], lambda e: e.matmul(ps.t[:, :], blk1.t[:, :], rk.t[:, tc_], start=True, stop=True))
                    kb.op("dve", [ps, v_], [bon],
                          lambda e: e.tensor_tensor(out=bon.t[:, tc_], in0=ps.t[:, :], in1=v_.t[:, tc_], op=ALU.mult))
                if STOPAT <= 4:
                    kb.barrier(); return
                for tb in range(NB):
                    cols = slice(tb * 128, (tb + 1) * 128)
                    for q4, src in enumerate((At, BtP, KtP, vb)):
                        kb.op("pe", [src, ident_bf], [psT],
                              lambda e: e.transpose(psT.t[:, q4 * 128:(q4 + 1) * 128], src.t[:, cols], ident_bf.t[:, :]))
                    kb.op("act", [psT], [tok], lambda e: e.activation(out=tok.t[:, tb, :], in_=psT.t[:, 0:512], func=AF.Copy))
                if STOPAT <= 5:
                    kb.barrier(); return
                yield_pair(kb, j, T, NB, NCK, At, Bt, Kt, Rt, tok, PCc, ApT_all, W_all, ArbT_all, ArkT_all, Hf, Hb_all,
                           mN, mNT, mIT, ident_f, S[0], BK, [S[1], S[3], S[4], S[5]], [BtP, vb], KtP)
                if STOPAT <= 7:
                    kb.barrier(); return
                y_sb = S[0]
                d_ = S[1]
                for tt in range(NTT):
                    tc_ = slice(tt * 512, (tt + 1) * 512)
                    ps = nextx()
                    kb.op("pe", [blk1, y_sb], [ps], lambda e: e.matmul(ps.t[:, :], blk1.t[:, :], y_sb.t[:, tc_], start=True, stop=True))
                    kb.op("dve", [ps, y_sb], [d_],
                          lambda e: e.scalar_tensor_tensor(out=d_.t[:, tc_], in0=ps.t[:, :], scalar=-1.0 / 64, in1=y_sb.t[:, tc_],
                                                           op0=ALU.mult, op1=ALU.add))
                    kb.op("act", [d_], [rn], lambda e: e.activation(out=rn.t[:, :], in_=d_.t[:, tc_], func=AF.Square))
                    ps2 = nextx()
                    kb.op("pe", [blk1, rn], [ps2], lambda e: e.matmul(ps2.t[:, :], blk1.t[:, :], rn.t[:, :], start=True, stop=True))
                    kb.op("act", [ps2], [rn],
                          lambda e: e.activation(out=rn.t[:, :], in_=ps2.t[:, :], func=AF.Sqrt, scale=1.0 / 64, bias=G["gn_eps"].t[:, 0:1]))
                    kb.op("dve", [rn], [rn], lambda e: e.reciprocal(out=rn.t[:, :], in_=rn.t[:, :]))
                    kb.op("dve", [d_, colp, rn], [d_],
                          lambda e: e.scalar_tensor_tensor(out=d_.t[:, tc_], in0=d_.t[:, tc_], scalar=cp(5), in1=rn.t[:, :],
                                                           op0=ALU.mult, op1=ALU.mult))
                    kb.op("dve", [d_, colp, bon], [d_],
                          lambda e: e.scalar_tensor_tensor(out=d_.t[:, tc_], in0=d_.t[:, tc_], scalar=cp(6), in1=bon.t[:, tc_],
                                                           op0=ALU.add, op1=ALU.add))
                    y = yo[tt % 2]
                    kb.op("dve", [d_, g_bf], [y],
                          lambda e: e.tensor_tensor(out=y.t[:, :], in0=d_.t[:, tc_], in1=g_bf.t[:, tc_], op=ALU.mult))
                    kb.dma("sp", yT_d[1536 + j * 128:1536 + (j + 1) * 128, seq_off + tt * 512: seq_off + (tt + 1) * 512], y.t[:, :],
                           r=[y], w=[ybuf])
    kb.barrier()


def yield_pair(kb, j, T, NB, NCK, At, Bt, Kt, Rt, tok, PCc, ApT_all, W_all, ArbT_all, ArkT_all, Hf, Hb_all, mN, mNT, mIT,
               ident_f, y_sb, BK, Sfree, Bfree, KtP):
    kb.barrier()
    with contextlib.ExitStack() as ec:
        NCTX = 7 if T >= 2048 else 2
        nv = T // 128
        fpool = [View(Sx.t[:, k * 128:(k + 1) * 128], f"fv{k}") for Sx in Sfree for k in range(nv)]
        bpool = [View(Bx.t[:, k * 128:(k + 1) * 128], f"bv{k}") for Bx in Bfree for k in range(nv)]
        banks = BK[0:7]
        ctx = []
        for g in range(NCTX):
            d = {}
            d["ps"] = [View(banks[g].t[:, q * 128:(q + 1) * 128], f"cps{g}_{q}", buf=banks[g].b) for q in range(4)]
            d["N"] = [fpool.pop() for _ in range(2)]
            d["M"] = [fpool.pop() for _ in range(2)]
            d["TT"] = [fpool.pop() for _ in range(2)]
            d["AakT"] = bpool.pop()
            d["W0"] = View(bpool.pop().t[:, 0:64], "cW0")
            d["TTb"] = bpool.pop()
            ctx.append(d)

        def cgen(d, hh, tb):
            pb = 64 * hh
            cols = slice(tb * 128, (tb + 1) * 128)
            ps = d["ps"]
            hs = slice(pb, pb + 64)
            kb.op("pe", [At, Bt], [ps[0]], lambda e: e.matmul(ps[0].t, At.t[hs, cols], Bt.t[hs, cols], start=True, stop=True))
            kb.op("pe", [At, Bt], [ps[1]], lambda e: e.matmul(ps[1].t, Bt.t[hs, cols], At.t[hs, cols], start=True, stop=True))
            kb.op("pe", [At, Kt], [ps[2]], lambda e: e.matmul(ps[2].t, Kt.t[hs, cols], At.t[hs, cols], start=True, stop=True))
            yield
            N0, M0, TT0 = d["N"][0], d["M"][0], d["TT"][0]
            kb.op("dve", [ps[0], mN], [N0], lambda e: e.tensor_tensor(out=N0.t[:, :], in0=ps[0].t, in1=mN.t[:, :], op=ALU.mult))
            kb.op("dve", [ps[1], mNT], [M0], lambda e: e.tensor_tensor(out=M0.t[:, :], in0=ps[1].t, in1=mNT.t[:, :], op=ALU.mult))
            kb.op("dve", [ps[2], mNT], [d["AakT"]],
                  lambda e: e.tensor_tensor(out=d["AakT"].t[:, :], in0=ps[2].t, in1=mNT.t[:, :], op=ALU.mult))
            kb.op(POOLENG, [M0, ident_f], [TT0],
                  lambda e: e.tensor_tensor(out=TT0.t[:, :], in0=M0.t[:, :], in1=ident_f.t[:, :], op=ALU.add))
            yield
            kb.op("pe", [Rt, Bt], [ps[3]], lambda e: e.matmul(ps[3].t, Bt.t[hs, cols], Rt.t[hs, cols], start=True, stop=True))
            kb.op("pe", [Rt, Kt], [ps[0]], lambda e: e.matmul(ps[0].t, Kt.t[hs, cols], Rt.t[hs, cols], start=True, stop=True))
            kb.op("pe", [d["AakT"], tok], [ps[1]],
                  lambda e: e.matmul(ps[1].t[:, 0:64], d["AakT"].t[:, :], tok.t[:, tb, 384 + pb:384 + pb + 64], start=True, stop=True))
            yield
            kb.op("dve", [ps[3], mIT], [ArbT_all],
                  lambda e: e.tensor_tensor(out=ArbT_all.t[:, tb, hh, :], in0=ps[3].t, in1=mIT.t[:, :], op=ALU.mult))
            kb.op("dve", [ps[0], mIT], [ArkT_all],
                  lambda e: e.tensor_tensor(out=ArkT_all.t[:, tb, hh, :], in0=ps[0].t, in1=mIT.t[:, :], op=ALU.mult))
            kb.op("act", [ps[1]], [d["W0"]], lambda e: e.activation(out=d["W0"].t[:, :], in_=ps[1].t[:, 0:64], func=AF.Copy))
            yield
            cur = 0
            for jj in range(1, 6):
                Np, Mp, TTp = d["N"][cur], d["M"][cur], d["TT"][cur]
                Nn, Mn, TTn = d["N"][1 - cur], d["M"][1 - cur], d["TT"][1 - cur]
                kb.op("pe", [Np, Mp], [ps[2]], lambda e: e.matmul(ps[2].t, Mp.t[:, :], Np.t[:, :], start=True, stop=True))
                if jj < 5:
                    kb.op("pe", [Np, Mp], [ps[3]], lambda e: e.matmul(ps[3].t, Np.t[:, :], Mp.t[:, :], start=True, stop=True))
                yield
                kb.op("act", [ps[2]], [Nn], lambda e: e.activation(out=Nn.t[:, :], in_=ps[2].t, func=AF.Copy))
                if jj < 5:
                    kb.op("act", [ps[3]], [Mn], lambda e: e.activation(out=Mn.t[:, :], in_=ps[3].t, func=AF.Copy))
                yield
                kb.op("pe", [Nn, TTp], [ps[0]], lambda e: e.matmul(ps[0].t, Nn.t[:, :], TTp.t[:, :], start=True, stop=True))
                yield
                kb.op("dve", [ps[0], TTp], [TTn],
                      lambda e: e.tensor_tensor(out=TTn.t[:, :], in0=ps[0].t, in1=TTp.t[:, :], op=ALU.add))
                yield
                cur = 1 - cur
            TTf = d["TT"][cur]
            kb.op("act", [TTf], [d["TTb"]], lambda e: e.activation(out=d["TTb"].t[:, :], in_=TTf.t[:, :], func=AF.Copy))
            yield
            kb.op("pe", [d["TTb"], d["W0"]], [ps[1]],
                  lambda e: e.matmul(ps[1].t[:, 0:64], d["TTb"].t[:, :], d["W0"].t[:, :], start=True, stop=True))
            kb.op("pe", [d["TTb"], tok], [ps[2]],
                  lambda e: e.matmul(ps[2].t[hs, :], tok.t[:, tb, pb:pb + 64], d["TTb"].t[:, :], start=True, stop=True))
            yield
            kb.op("act", [ps[1]], [W_all], lambda e: e.activation(out=W_all.t[:, tb, hh, :], in_=ps[1].t[:, 0:64], func=AF.Copy))
            kb.op("act", [ps[2]], [ApT_all], lambda e: e.activation(out=ApT_all.t[hs, tb, :], in_=ps[2].t[hs, :], func=AF.Copy))
            yield

        jobs = [(hh, tb) for tb in range(NB) for hh in range(2)]
        for g0 in range(0, len(jobs), NCTX):
            run_lockstep([cgen(ctx[g], *jobs[g0 + g]) for g in range(min(NCTX, len(jobs) - g0))])

    if STOPAT <= 6:
        return
    with contextlib.ExitStack() as ed:
        U_sb = [View(KtP.t[:, k * 64:(k + 1) * 64], f"U_sb{k}") for k in range(2)]
        kb.op("dve", [], [Hf], lambda e: e.memset(Hf.t[:, :], 0.0))
        kb.op("dve", [], [Hb_all], lambda e: e.memset(Hb_all.t[:, 0, :], 0.0))
        HfV = [View(Hf.t[64 * hh:64 * hh + 64, :], f"Hf{hh}") for hh in range(2)]
        HbV = [View(Hb_all.t[64 * hh:64 * hh + 64, :, :], f"Hb{hh}") for hh in range(2)]
        for hh in range(2):
            HfV[hh].b.writers = dict(Hf.b.writers)
            HbV[hh].b.writers = dict(Hb_all.b.writers)

        def dgen(hh):
            pb = 64 * hh
            hs = slice(pb, pb + 64)
            psU = View(BK[0 + hh].t[:, 0:64], f"psU{hh}", buf=BK[0 + hh].b)
            psH = View(BK[2 + hh].t[:, 0:64], f"psH{hh}", buf=BK[2 + hh].b)
            psY = View(BK[4 + hh].t[:, 0:128], f"psY{hh}", buf=BK[4 + hh].b)
            Us = U_sb[hh]
            for tb in range(NB):
                for cc in range(2):
                    ck = 2 * tb + cc
                    tp = slice(64 * cc, 64 * cc + 64)
                    kb.op("pe", [ApT_all, HbV[hh]], [psU],
                          lambda e: e.matmul(psU.t[tp, :], ApT_all.t[hs, tb, 64 * cc:64 * cc + 64], Hb_all.t[hs, ck, :],
                                             start=True, stop=True))
                    yield
                    kb.op("dve", [psU, W_all], [Us],
                          lambda e: e.tensor_tensor(out=Us.t[tp, :], in0=psU.t[tp, :], in1=W_all.t[tp, tb, hh, :], op=ALU.add))
                    yield
                    kb.op("pe", [tok], [psH],
                          lambda e: e.matmul(psH.t[hs, :], tok.t[tp, tb, 256 + pb:256 + pb + 64], tok.t[tp, tb, 384 + pb:384 + pb + 64],
                                             start=True, stop=False))
                    kb.op("pe", [tok, Us], [psH],
                          lambda e: e.matmul(psH.t[hs, :], tok.t[tp, tb, 128 + pb:128 + pb + 64], Us.t[tp, :], start=False, stop=True))
                    yield
                    kb.op("dve", [psH, HfV[hh], PCc], [HfV[hh]],
                          lambda e: e.scalar_tensor_tensor(out=Hf.t[hs, :], in0=Hf.t[hs, :], scalar=PCc.t[hs, ck:ck + 1],
                                                           in1=psH.t[hs, :], op0=ALU.mult, op1=ALU.add))
                    yield
                    kb.op("act", [HfV[hh]], [HbV[hh]],
                          lambda e: e.activation(out=Hb_all.t[hs, ck + 1, :], in_=Hf.t[hs, :], func=AF.Copy))
                    yield
                cols = slice(tb * 128, (tb + 1) * 128)
                kb.op("pe", [Us, ArbT_all], [psY],
                      lambda e: e.matmul(psY.t[hs, :], Us.t[:, :], ArbT_all.t[:, tb, hh, :], start=True, stop=False))
                kb.op("pe", [tok, ArkT_all], [psY],
                      lambda e: e.matmul(psY.t[hs, :], tok.t[:, tb, 384 + pb:384 + pb + 64], ArkT_all.t[:, tb, hh, :],
                                         start=False, stop=False))
                for cc in range(2):
                    kb.op("pe", [HbV[hh], Rt], [psY],
                          lambda e: e.matmul(psY.t[hs, 64 * cc:64 * cc + 64], Hb_all.t[hs, 2 * tb + cc, :],
                                             Rt.t[hs, tb * 128 + 64 * cc: tb * 128 + 64 * cc + 64], start=False, stop=(cc == 1)))
                yield
                kb.op("act", [psY], [y_sb], lambda e: e.activation(out=y_sb.t[hs, cols], in_=psY.t[hs, :], func=AF.Copy))
                yield

        run_lockstep([dgen(0), dgen(1)])
    kb.barrier()


CONST_NAMES = ["c_rot_ret", "c_rot_dsa", "c_cs_ret", "c_cs_dsa", "c_ret_maskT", "c_ret_qdec", "c_ret_kdec", "c_ident",
               "c_negmask", "c_maskN", "c_maskNT", "c_maskIT", "c_blockones"]


def declare_consts(nc, HC):
    C = {}
    for n in CONST_NAMES:
        C[n] = nc.dram_tensor(n, list(HC[n].shape), F32, kind="ExternalInput").ap()
    C["ret_cd"] = HC["ret_cd"]
    return C


def make_globals(kb, es, C):
    G = {}
    G["ones_f"] = kb.sb(es, [128, 128], F32, "ones_f")
    kb.op("dve", [], [G["ones_f"]], lambda e: e.memset(G["ones_f"].t[:], 1.0))
    G["ident_bf"] = kb.sb(es, [128, 128], BF16, "ident_bf")
    kb.dma("pool", G["ident_bf"].t[:, :], C["c_ident"], r=[], w=[G["ident_bf"]])
    G["ident_f"] = kb.sb(es, [128, 128], F32, "ident_f")
    kb.dma("sp", G["ident_f"].t[:, :], C["c_ident"], r=[], w=[G["ident_f"]])
    init_eps(kb, es)
    G["gn_eps"] = kb.sb(es, [128, 1], F32, "gn_eps")
    kb.op("dve", [], [G["gn_eps"]], lambda e: e.memset(G["gn_eps"].t[:], 64e-5))
    return G


OFF_GATE = 5828


def rms_hT_cols(kb, xt, gcol, hT, hcol0, sqs, ps_ss, rs, G, W):
    ones = G["ones_f"]
    for c in range(8):
        sq = sqs[c % 2]
        kb.op("act", [xt], [sq], lambda e: e.activation(out=sq.t[:, :W], in_=xt.t[:, c, :W], func=AF.Square))
        kb.op("pe", [sq, ones], [ps_ss],
              lambda e: e.matmul(ps_ss.t[:, :W], ones.t[:], sq.t[:, :W], start=(c == 0), stop=(c == 7)))
    kb.op("act", [ps_ss], [rs],
          lambda e: e.activation(out=rs.t[:, :W], in_=ps_ss.t[:, :W], func=AF.Sqrt, scale=1.0 / D, bias=eps_ap(kb)))
    kb.op("dve", [rs], [rs], lambda e: e.reciprocal(out=rs.t[:, :W], in_=rs.t[:, :W]))
    for c in range(8):
        kb.op("dve", [xt, rs, gcol], [hT],
              lambda e: e.scalar_tensor_tensor(out=hT.t[:, c, hcol0:hcol0 + W], in0=xt.t[:, c, :W], scalar=gcol.t[:, c:c + 1],
                                               in1=rs.t[:, :W], op0=ALU.mult, op1=ALU.mult))


def stage_hT(kb, xT_d, xbufs, tile0, T, g_d, hT, G):
    with contextlib.ExitStack() as es:
        gcol = kb.sb(es, [128, 8], F32, "gcol")
        kb.dma("sp", gcol.t[:, :], g_d.rearrange("(c p) -> p c", p=128), r=[], w=[gcol], allow_slow_non_contiguous=True)
        xts = [kb.sb(es, [128, 8, 512], F32, "xt") for _ in range(2)]
        sqs = [kb.sb(es, [128, 512], F32, "sq") for _ in range(2)]
        rs = kb.sb(es, [128, 512], F32, "rs")
        ps_ss = kb.ps(es, [128, 512], F32, "ps_ss")
        for tt in range(T // 512):
            xt = xts[tt % 2]
            ti = tile0 + tt
            kb.dma("sp", xt.t[:, :, :], xT_d[:, :, ti * 512:(ti + 1) * 512], r=[xbufs[ti]], w=[xt])
            rms_hT_cols(kb, xt, gcol, hT, tt * 512, sqs, ps_ss, rs, G, 512)
    kb.barrier()


def stage_merge(kb, hT, T, tile0, xT_d, xbufs, yT_d, ybuf, w_d, p_ret_d, p_dsa_d, p_rwkv_d, w_out_d, G):
    with contextlib.ExitStack() as es:
        wg = kb.sb(es, [128, 8, 3072], BF16, "wgate")
        wgs = [wg, wg, wg]

        def load_gate(s3):
            for c in range(8):
                kb.dma("pool", wg.t[:, c, s3 * 1024:(s3 + 1) * 1024],
                       w_d[c * 128:(c + 1) * 128, OFF_GATE + s3 * 1024:OFF_GATE + (s3 + 1) * 1024], r=[], w=[wg])

        load_gate(0)
        pr = kb.sb(es, [128, 8, 1024], BF16, "p_ret")
        for c in range(8):
            kb.dma("pool", pr.t[:, c, :], p_ret_d[c * 128:(c + 1) * 128, :], r=[], w=[pr])
        load_gate(1)
        pd = kb.sb(es, [64, 8, 1024], BF16, "p_dsa")
        for h in range(8):
            kb.dma("pool", pd.t[:, h, :], p_dsa_d[h * 64:(h + 1) * 64, :], r=[], w=[pd])
        load_gate(2)
        pw = kb.sb(es, [128, 4, 1024], BF16, "p_rwkv")
        for c in range(4):
            kb.dma("pool", pw.t[:, c, :], p_rwkv_d[c * 128:(c + 1) * 128, :], r=[], w=[pw])
        wo = kb.sb(es, [128, 8, 1024], BF16, "w_out")
        for c in range(8):
            kb.dma("pool", wo.t[:, c, :], w_out_d[c * 128:(c + 1) * 128, :], r=[], w=[wo])
        yr = [kb.sb(es, [128, 8, 512], BF16, "yr") for _ in range(2)]
        yd = [kb.sb(es, [64, 8, 512], BF16, "yd")] * 2
        yw = [kb.sb(es, [128, 4, 512], BF16, "yw")] * 2
        xts = [kb.sb(es, [128, 8, 512], F32, "xt")] * 2
        mg = kb.sb(es, [128, 8, 512], BF16, "merged")
        sgs = [kb.sb(es, [128, 512], F32, "sg") for _ in range(2)]
        macc = kb.sb(es, [128, 512], F32, "macc")
        tmp = kb.sb(es, [128, 512], F32, "tmp")
        psg = [kb.ps(es, [128, 512], F32, "psg") for _ in range(2)]
        psp = [kb.ps(es, [128, 512], F32, "psp") for _ in range(2)]
        pso = [kb.ps(es, [128, 512], F32, "pso") for _ in range(2)]
        it = 0
        for tt in range(T // 512):
            ti = tile0 + tt
            gcols = slice(ti * 512, (ti + 1) * 512)
            hc = slice(tt * 512, (tt + 1) * 512)
            a, b_, c_, xt = yr[tt % 2], yd[tt % 2], yw[tt % 2], xts[tt % 2]
            kb.dma("sp", a.t[:, :, :], yT_d[0:1024, gcols].rearrange("(c p) t -> p c t", p=128), r=[ybuf], w=[a])
            kb.dma("sp", b_.t[:, :, :], yT_d[1024:1536, gcols].rearrange("(h d) t -> d h t", d=64), r=[ybuf], w=[b_])
            kb.dma("sp", c_.t[:, :, :], yT_d[1536:2048, gcols].rearrange("(c p) t -> p c t", p=128), r=[ybuf], w=[c_])
            kb.dma("sp", xt.t[:, :, :], xT_d[:, :, gcols], r=[xbufs[ti]], w=[xt])
            for db in range(8):
                dsl = slice(db * 128, (db + 1) * 128)
                for br in range(3):
                    it += 1
                    pg, pp, sg = psg[it % 2], psp[it % 2], sgs[it % 2]
                    kb.op("pe", [wg, hT], [pg],
                          lambda e: [e.matmul(pg.t[:, :], wg.t[:, c, br * 1024 + db * 128: br * 1024 + (db + 1) * 128],
                                              hT.t[:, c, hc], start=(c == 0), stop=(c == 7)) for c in range(8)][-1])
                    kb.op("act", [pg], [sg], lambda e: e.activation(out=sg.t[:, :], in_=pg.t[:, :], func=AF.Sigmoid))
                    if br == 0:
                        kb.op("pe", [pr, a], [pp],
                              lambda e: [e.matmul(pp.t[:, :], pr.t[:, c, dsl], a.t[:, c, :], start=(c == 0), stop=(c == 7))
                                         for c in range(8)][-1])
                    elif br == 1:
                        kb.op("pe", [pd, b_], [pp],
                              lambda e: [e.matmul(pp.t[:, :], pd.t[:, c, dsl], b_.t[:, c, :], start=(c == 0), stop=(c == 7))
                                         for c in range(8)][-1])
                    else:
                        kb.op("pe", [pw, c_], [pp],
                              lambda e: [e.matmul(pp.t[:, :], pw.t[:, c, dsl], c_.t[:, c, :], start=(c == 0), stop=(c == 3))
                                         for c in range(4)][-1])
                    if br == 0:
                        kb.op("dve", [sg, pp], [macc],
                              lambda e: e.tensor_tensor(out=macc.t[:, :], in0=sg.t[:, :], in1=pp.t[:, :], op=ALU.mult))
                    else:
                        kb.op("dve", [sg, pp], [tmp],
                              lambda e: e.tensor_tensor(out=tmp.t[:, :], in0=sg.t[:, :], in1=pp.t[:, :], op=ALU.mult))
                        if br == 1:
                            kb.op("dve", [tmp, macc], [macc],
                                  lambda e: e.tensor_tensor(out=macc.t[:, :], in0=macc.t[:, :], in1=tmp.t[:, :], op=ALU.add))
                        else:
                            kb.op("dve", [tmp, macc], [mg],
                                  lambda e: e.tensor_tensor(out=mg.t[:, db, :], in0=macc.t[:, :], in1=tmp.t[:, :], op=ALU.add))
            for ob in range(8):
                po = pso[ob % 2]
                kb.op("pe", [wo, mg], [po],
                      lambda e: [e.matmul(po.t[:, :], wo.t[:, db, ob * 128:(ob + 1) * 128], mg.t[:, db, :],
                                          start=(db == 0), stop=(db == 7)) for db in range(8)][-1])
                kb.op("dve", [po, xt], [xt],
                      lambda e: e.tensor_tensor(out=xt.t[:, ob, :], in0=po.t[:, :], in1=xt.t[:, ob, :], op=ALU.add))
            kb.dma("sp", xT_d[:, :, gcols], xt.t[:, :, :], r=[xt], w=[xbufs[ti]])
    kb.barrier()


def stage_in(kb, x_d, xT_d, xbufs, NT, G):
    ident = G["ident_f"]
    with contextlib.ExitStack() as es:
        xin = [kb.sb(es, [128, 1024], F32, "xin") for _ in range(2)]
        xo = [kb.sb(es, [128, 8, 128], F32, "xo") for _ in range(2)]
        pst = [kb.ps(es, [128, 512], F32, "pst") for _ in range(2)]
        for tb in range(NT // 128):
            xi, xo_ = xin[tb % 2], xo[tb % 2]
            kb.dma("sp", xi.t[:, :], x_d[tb * 128:(tb + 1) * 128, :], r=[], w=[xi])
            for half in range(2):
                pt = pst[half]
                for q in range(4):
                    c = half * 4 + q
                    kb.op("pe", [xi, ident], [pt],
                          lambda e: e.transpose(pt.t[:, q * 128:(q + 1) * 128], xi.t[:, c * 128:(c + 1) * 128], ident.t[:, :]))
                kb.op("act" if half == 0 else "dve", [pt], [xo_],
                      (lambda e: e.activation(out=xo_.t[:, 0:4, :].rearrange("p a b -> p (a b)"), in_=pt.t[:, :], func=AF.Copy))
                      if half == 0 else
                      (lambda e: e.tensor_copy(out=xo_.t[:, 4:8, :].rearrange("p a b -> p (a b)"), in_=pt.t[:, :])))
            kb.dma("sp", xT_d[:, :, tb * 128:(tb + 1) * 128], xo_.t[:, :, :], r=[xo_], w=[xbufs[tb // 4]])
    kb.barrier()


def stage_out(kb, xT_d, xbufs, NT, g_d, out_d, G):
    ident = G["ident_f"]
    with contextlib.ExitStack() as es:
        gcol = kb.sb(es, [128, 8], F32, "gcol")
        kb.dma("sp", gcol.t[:, :], g_d.rearrange("(c p) -> p c", p=128), r=[], w=[gcol], allow_slow_non_contiguous=True)
        xts = [kb.sb(es, [128, 8, 512], F32, "xt") for _ in range(2)]
        sqs = [kb.sb(es, [128, 512], F32, "sq") for _ in range(2)]
        rs = kb.sb(es, [128, 512], F32, "rs")
        hf = kb.sb(es, [128, 8, 512], F32, "hf")
        ot = [kb.sb(es, [128, 1024], F32, "ot") for _ in range(2)]
        ps_ss = kb.ps(es, [128, 512], F32, "ps_ss")
        pst = [kb.ps(es, [128, 512], F32, "pst") for _ in range(2)]
        for ti in range(NT // 512):
            xt = xts[ti % 2]
            kb.dma("sp", xt.t[:, :, :], xT_d[:, :, ti * 512:(ti + 1) * 512], r=[xbufs[ti]], w=[xt])
            rms_hT_cols(kb, xt, gcol, hf, 0, sqs, ps_ss, rs, G, 512)
            for tb in range(4):
                o_ = ot[tb % 2]
                for half in range(2):
                    pt = pst[half]
                    for q in range(4):
                        c = half * 4 + q
                        kb.op("pe", [hf, ident], [pt],
                              lambda e: e.transpose(pt.t[:, q * 128:(q + 1) * 128], hf.t[:, c, tb * 128:(tb + 1) * 128], ident.t[:, :]))
                    if half == 0:
                        kb.op("act", [pt], [o_], lambda e: e.activation(out=o_.t[:, 0:512], in_=pt.t[:, :], func=AF.Copy))
                    else:
                        kb.op("dve", [pt], [o_], lambda e: e.tensor_copy(out=o_.t[:, 512:1024], in_=pt.t[:, :]))
                r0 = ti * 512 + tb * 128
                kb.dma("sp", out_d[r0:r0 + 128, :], o_.t[:, :], r=[o_], w=[])
    kb.barrier()


RWKV_NAMES = ['rwkv_mu', 'rwkv_w0', 'rwkv_w2', 'rwkv_a0', 'rwkv_a2', 'rwkv_g2', 'rwkv_k_k', 'rwkv_k_a', 'rwkv_r_k', 'rwkv_ln_w',
              'rwkv_ln_b']
W_SHAPES = {
    'norm_ffa': [1024], 'ffa_w_in': [1024, 5632], 'ffa_w_out': [2816, 1024], 'norm_mix': [1024], 'w_in': [1024, 8900],
    'rwkv_mu': [1792], 'rwkv_w0': [512], 'rwkv_w2': [64, 512], 'rwkv_a0': [512], 'rwkv_a2': [64, 512], 'rwkv_g2': [128, 512],
    'rwkv_k_k': [512], 'rwkv_k_a': [512], 'rwkv_r_k': [512], 'rwkv_ln_w': [512], 'rwkv_ln_b': [512],
    'p_ret': [1024, 1024], 'p_dsa': [512, 1024], 'p_rwkv': [512, 1024], 'w_out': [1024, 1024], 'norm_ffb': [1024],
    'ffb_w_in': [1024, 5632], 'ffb_w_out': [2816, 1024],
}


def build_program(T, NSEQ, DEPTH, HC, debug_y=False):
    NT = NSEQ * T
    nc = bass.Bass("TRN2", target_bir_lowering=False)
    x_d = nc.dram_tensor("x", [NT, 1024], F32, kind="ExternalInput").ap()
    WD = {}
    for n, shp in W_SHAPES.items():
        WD[n] = nc.dram_tensor(n, [DEPTH] + shp, F32, kind="ExternalInput").ap()
    gfin = nc.dram_tensor("norm_final", [1024], F32, kind="ExternalInput").ap()
    C = {}
    for n in CONST_NAMES:
        C[n] = nc.dram_tensor(n, list(HC[n].shape), F32, kind="ExternalInput").ap()
    C["ret_cd"] = HC["ret_cd"]
    out_d = nc.dram_tensor("out", [NT, 1024], F32, kind="ExternalOutput").ap()
    xT_t = nc.dram_tensor("xT_scr", [1024, NT], F32, kind="Internal").ap()
    if debug_y:
        yT_d = nc.dram_tensor("yT_scr", [2048, NT], BF16, kind="ExternalOutput").ap()
    else:
        yT_d = nc.dram_tensor("yT_scr", [2048, NT], BF16, kind="Internal").ap()
    xT_d = xT_t.rearrange("(c p) t -> p c t", p=128)
    kb = KB(nc)
    with kb.root:
        es = kb.root
        G = make_globals(kb, es, C)
        xbufs = [Buf(f"xb{i}") for i in range(NT // 512)]
        ybuf = Buf("ybuf")
        stage_in(kb, x_d, xT_d, xbufs, NT, G)
        for l in range(DEPTH):
            stage_ffn(kb, xT_d, xbufs, NT, WD['ffa_w_in'][l], WD['ffa_w_out'][l], WD['norm_ffa'][l], G)
            PL = {n: WD[n][l] for n in RWKV_NAMES}
            for s in range(NSEQ):
                with contextlib.ExitStack() as em:
                    hT = kb.sb(em, [128, 8, T], BF16, "hT")
                    stage_hT(kb, xT_d, xbufs, s * (T // 512), T, WD['norm_mix'][l], hT, G)
                    stage_retention(kb, hT, T, s * T, WD['w_in'][l], yT_d, ybuf, C, G)
                    stage_dsa(kb, hT, T, s * T, WD['w_in'][l], yT_d, ybuf, C, G)
                    stage_rwkv(kb, hT, T, s * T, WD['w_in'][l], PL, yT_d, ybuf, C, G)
                    stage_merge(kb, hT, T, s * (T // 512), xT_d, xbufs, yT_d, ybuf, WD['w_in'][l], WD['p_ret'][l],
                                WD['p_dsa'][l], WD['p_rwkv'][l], WD['w_out'][l], G)
                kb.barrier()
            stage_ffn(kb, xT_d, xbufs, NT, WD['ffb_w_in'][l], WD['ffb_w_out'][l], WD['norm_ffb'][l], G)
        stage_out(kb, xT_d, xbufs, NT, gfin, out_d, G)
        kb.finish()
    return nc, kb


def kernel(**inputs):
    T, NSEQ, DEPTH, NCORE = 2048, 2, 2, 8
    HC = host_consts(T)
    nc, kb = build_program(T, NSEQ, DEPTH, HC)
    shared = {}
    for n, shp in W_SHAPES.items():
        shared[n] = np.ascontiguousarray(np.asarray(inputs[n], dtype=np.float32)).reshape([DEPTH] + shp)
    shared["norm_final"] = np.ascontiguousarray(np.asarray(inputs["norm_final"], dtype=np.float32))
    for n in CONST_NAMES:
        shared[n] = HC[n]
    x = np.asarray(inputs["x"], dtype=np.float32)
    in_maps = []
    for c in range(NCORE):
        m = dict(shared)
        m["x"] = np.ascontiguousarray(x[c * NSEQ:(c + 1) * NSEQ]).reshape(NSEQ * T, 1024)
        in_maps.append(m)
    res = run_bass_kernel_spmd(nc, in_maps, core_ids=list(range(NCORE)))
    outs = [np.asarray(r["out"], dtype=np.float32).reshape(NSEQ, T, 1024) for r in res.results]
    return np.concatenate(outs, axis=0)
```

```python
import contextlib
import numpy as np
import concourse.bass as bass
import concourse.mybir as mybir
from concourse.bass_utils import run_bass_kernel_spmd

F32 = mybir.dt.float32
BF16 = mybir.dt.bfloat16
AF = mybir.ActivationFunctionType
ALU = mybir.AluOpType
AX = mybir.AxisListType

ND = 40
ND_SP = 24


class Buf:
    __slots__ = ("name", "writers", "readers", "excl")

    def __init__(self, name=""):
        self.name = name
        self.excl = False
        self.writers = {}
        self.readers = {}

    @property
    def last_w(self):
        return dict(self.writers)

    @last_w.setter
    def last_w(self, v):
        self.writers = dict(v) if isinstance(v, dict) else ({v[0]: v[1]} if v else {})


class Tile:
    def __init__(self, t, name, nsub=1):
        self.t = t
        self.b = Buf(name)
        self.subs = [Buf(f"{name}.{i}") for i in range(nsub)] if nsub > 1 else None

    def __getitem__(self, idx):
        return self.t[idx]


class KB:
    def __init__(self, nc):
        self.nc = nc
        self.root = contextlib.ExitStack()
        self.E = {"pe": nc.tensor, "act": nc.scalar, "dve": nc.vector, "pool": nc.gpsimd, "sp": nc.sync}
        self.sem = {n: self.root.enter_context(nc.semaphore("s_" + n)) for n in self.E}
        self.cnt = {n: 0 for n in self.E}
        self.seen = {n: {} for n in self.E}
        self.dsem = [self.root.enter_context(nc.semaphore(f"d_{i}")) for i in range(ND)]
        self.dcnt = [0] * ND
        self.dnext = 0
        self.dnext_pool = ND_SP
        self.nwaits = 0
        self.ninst = 0
        self.uid = 0

    def sb(self, es, shape, dtype, name=None, nsub=1):
        self.uid += 1
        name = f"{name or 't'}_{self.uid}"
        t = es.enter_context(self.nc.sbuf_tensor(name, list(shape), dtype))
        return Tile(t, name, nsub)

    def ps(self, es, shape, dtype, name=None):
        self.uid += 1
        name = f"{name or 'p'}_{self.uid}"
        t = es.enter_context(self.nc.psum_tensor(name, list(shape), dtype))
        tl = Tile(t, name)
        tl.b.excl = True
        return tl

    def _semof(self, key):
        return self.sem[key] if isinstance(key, str) else self.dsem[key[1]]

    def _wait(self, eng, tok):
        if tok is None:
            return
        key, val = tok
        if self.seen[eng].get(key, 0) >= val:
            return
        self.E[eng].wait_ge(self._semof(key), val)
        self.seen[eng][key] = val
        self.nwaits += 1

    def _deps(self, eng, r, w, accum=False):
        for b in r:
            for k, v in b.writers.items():
                self._wait(eng, (k, v))
        for b in w:
            for k, v in b.writers.items():
                if accum and not isinstance(k, str) and not b.readers:
                    continue
                if k != eng or eng != "pe":
                    self._wait(eng, (k, v))
            for k, v in b.readers.items():
                if k != eng or eng != "pe":
                    self._wait(eng, (k, v))

    def _record(self, tok, r, w, accum=False):
        k, v = tok
        for b in r:
            b.readers[k] = v
        for b in w:
            if accum and not b.readers:
                b.writers[k] = v
            else:
                b.writers = {k: v}
            b.readers = {}

    @staticmethod
    def _bufs(xs):
        out = []
        for x in xs:
            if isinstance(x, Tile):
                out.append(x.b)
            elif isinstance(x, Buf):
                out.append(x)
            elif x is None:
                pass
            else:
                raise TypeError(x)
        return out

    def op(self, eng, r, w, emit):
        r = self._bufs(r)
        w = self._bufs(w)
        for b in r:
            if b.excl and b not in w:
                w = w + [b]
        self._deps(eng, r, w)
        ins = emit(self.E[eng])
        self.cnt[eng] += 1
        ins.then_inc(self.sem[eng], 1)
        self.ninst += 1
        self._record((eng, self.cnt[eng]), r, w)

    def dma(self, q, out, in_, r, w, **kw):
        r = self._bufs(r)
        w = self._bufs(w)
        self._deps(q, r, w, accum=True)
        if q == "pool":
            i = self.dnext_pool
            self.dnext_pool = ND_SP + (i + 1 - ND_SP) % (ND - ND_SP)
        else:
            i = self.dnext
            self.dnext = (i + 1) % ND_SP
        if self.dcnt[i] > 0:
            self._wait(q, (("d", i), self.dcnt[i]))
        self.dcnt[i] += 16
        ins = self.E[q].dma_start(out=out, in_=in_, **kw)
        ins.then_inc(self.dsem[i], 16)
        self.ninst += 1
        self._record((("d", i), self.dcnt[i]), r, w, accum=True)

    def barrier(self, engines=None):
        for e in (engines or self.E):
            for k in self.E:
                if k != e and self.cnt[k] > 0:
                    self._wait(e, (k, self.cnt[k]))
            for i in range(ND):
                if self.dcnt[i] > 0:
                    self._wait(e, (("d", i), self.dcnt[i]))

    def finish(self):
        self.barrier(engines=["sp"])


class View:
    def __init__(self, ap, name="", buf=None):
        self.t = ap
        self.b = buf if buf is not None else Buf(name)


def _bufs_ext(xs):
    out = []
    for x in xs:
        if isinstance(x, (Tile, View)):
            out.append(x.b)
        elif isinstance(x, Buf):
            out.append(x)
        elif x is None:
            pass
        else:
            raise TypeError(x)
    return out


KB._bufs = staticmethod(_bufs_ext)

import numpy as np


def host_consts(T=2048):
    c = {}
    m = np.arange(128)
    d = m % 64
    P = np.zeros((128, 128), np.float32)
    part = np.where(d < 32, m + 32, m - 32)
    P[part, m] = 1.0
    c["c_rot_ret"] = P
    P2 = np.zeros((128, 128), np.float32)
    for mm in range(128):
        dd = mm % 64
        if dd < 8:
            P2[mm + 8, mm] = 1.0
        elif dd < 16:
            P2[mm - 8, mm] = 1.0
    c["c_rot_dsa"] = P2
    t = np.arange(T, dtype=np.float64)
    inv = 10000.0 ** (-np.arange(0, 64, 2, dtype=np.float64) / 64)
    ang = t[None, :] * inv[d % 32][:, None]
    sgn = np.where(d < 32, -1.0, 1.0)[:, None]
    c["c_cs_ret"] = np.stack([np.cos(ang), np.sin(ang) * sgn]).astype(np.float32)
    inv2 = 500000.0 ** (-np.arange(0, 16, 2, dtype=np.float64) / 16)
    cos2 = np.ones((128, T)); sin2 = np.zeros((128, T))
    for mm in range(128):
        dd = mm % 64
        if dd < 16:
            a = t * inv2[dd % 8]
            cos2[mm] = np.cos(a)
            sin2[mm] = np.sin(a) * (-1.0 if dd < 8 else 1.0)
    c["c_cs_dsa"] = np.stack([cos2, sin2]).astype(np.float32)
    gam = 1.0 - 2.0 ** (-5.0 - np.arange(8, dtype=np.float64))
    j = np.arange(128)[:, None]; i = np.arange(128)[None, :]
    maskT = np.zeros((128, 8, 128), np.float64)
    for h in range(8):
        maskT[:, h, :] = np.where(i >= j, gam[h] ** np.maximum(i - j, 0), 0.0) / 8.0
    c["c_ret_maskT"] = maskT.astype(np.float32)
    qdec = np.zeros((128, 4, 128), np.float64)
    for p in range(4):
        for mm in range(128):
            h = 2 * p + mm // 64
            qdec[mm, p, :] = gam[h] ** (np.arange(128) + 1.0)
    c["c_ret_qdec"] = qdec.astype(np.float32)
    kdec = np.zeros((128, 8), np.float64)
    for h in range(8):
        kdec[:, h] = gam[h] ** (127.0 - np.arange(128)) / 8.0
    c["c_ret_kdec"] = kdec.astype(np.float32)
    c["ret_cd"] = [float(g ** 128) for g in gam]
    c["c_ident"] = np.eye(128, dtype=np.float32)
    c["c_negmask"] = np.where(i <= j, 0.0, -1e30).astype(np.float32)
    tau = np.arange(128)[:, None]; sig = np.arange(128)[None, :]
    same = (tau // 64) == (sig // 64)
    c["c_maskN"] = (same & (sig < tau)).astype(np.float32)
    c["c_maskNT"] = c["c_maskN"].T.copy()
    c["c_maskIT"] = (same & (sig <= tau)).astype(np.float32).T.copy()
    c["c_blockones"] = ((m[:, None] // 64) == (m[None, :] // 64)).astype(np.float32)
    return c


D = 1024
DFF = 2816
EPS = 1e-6


def make_consts(kb, es):
    c = {}
    c["ones_f"] = kb.sb(es, [128, 128], F32, "ones_f")
    kb.op("dve", [], [c["ones_f"]], lambda e: e.memset(c["ones_f"].t[:], 1.0))
    return c


def rms_hT(kb, xt, gcol, hT, sqs, ps_ss, rs, consts, W, eps=EPS):
    ones = consts["ones_f"]
    for c in range(8):
        sq = sqs[c % 2]
        kb.op("act", [xt], [sq], lambda e: e.activation(out=sq.t[:, :W], in_=xt.t[:, c, :W], func=AF.Square))
        kb.op("pe", [sq, ones], [ps_ss],
              lambda e: e.matmul(ps_ss.t[:, :W], ones.t[:], sq.t[:, :W], start=(c == 0), stop=(c == 7)))
    kb.op("act", [ps_ss], [rs],
          lambda e: e.activation(out=rs.t[:, :W], in_=ps_ss.t[:, :W], func=AF.Sqrt, scale=1.0 / D, bias=eps_ap(kb)))
    kb.op("dve", [rs], [rs], lambda e: e.reciprocal(out=rs.t[:, :W], in_=rs.t[:, :W]))
    for c in range(8):
        kb.op("dve", [xt, rs, gcol], [hT],
              lambda e: e.scalar_tensor_tensor(out=hT.t[:, c, :W], in0=xt.t[:, c, :W], scalar=gcol.t[:, c:c + 1],
                                               in1=rs.t[:, :W], op0=ALU.mult, op1=ALU.mult))


_eps = {}


def eps_ap(kb):
    return _eps[id(kb)].t[:, 0:1]


def init_eps(kb, es):
    t = kb.sb(es, [128, 1], F32, "epsc")
    kb.op("dve", [], [t], lambda e: e.memset(t.t[:], EPS))
    _eps[id(kb)] = t


def load_w_bf16(kb, dst, src_d, nchunk, ncols, split=2):
    step = ncols // split
    for c in range(nchunk):
        for s in range(split):
            kb.dma("pool", dst.t[:, c, s * step:(s + 1) * step], src_d[c * 128:(c + 1) * 128, s * step:(s + 1) * step],
                   r=[], w=[dst])


def stage_ffn(kb, xT_d, xbufs, NT, w_in_d, w_out_d, g_d, consts, TT=512):
    nc = kb.nc
    with contextlib.ExitStack() as es:
        w1 = kb.sb(es, [128, 8, 2 * DFF], BF16, "w1")
        w2 = kb.sb(es, [128, 22, D], BF16, "w2")
        gcol = kb.sb(es, [128, 8], F32, "gcol")
        xts = [kb.sb(es, [128, 8, TT], F32, "xt") for _ in range(2)]
        sqs = [kb.sb(es, [128, TT], F32, "sq") for _ in range(2)]
        rs = kb.sb(es, [128, TT], F32, "rs")
        hT = kb.sb(es, [128, 8, TT], BF16, "hT")
        aT = kb.sb(es, [128, 22, TT], BF16, "aT")
        sgs = [kb.sb(es, [128, TT], BF16, "sg") for _ in range(2)]
        ps_ss = kb.ps(es, [128, TT], F32, "ps_ss")
        psg = [kb.ps(es, [128, TT], F32, "psg") for _ in range(2)]
        psu = [kb.ps(es, [128, TT], F32, "psu") for _ in range(2)]
        pso = [kb.ps(es, [128, TT], F32, "pso") for _ in range(2)]

        kb.dma("sp", gcol.t[:, :], g_d.rearrange("(c p) -> p c", p=128), r=[], w=[gcol],
               allow_slow_non_contiguous=True)
        load_w_bf16(kb, w1, w_in_d, 8, 2 * DFF)
        load_w_bf16(kb, w2, w_out_d, 22, D)

        for ti in range(NT // TT):
            xt = xts[ti % 2]
            sl = slice(ti * TT, (ti + 1) * TT)
            kb.dma("sp", xt.t[:, :, :], xT_d[:, :, sl], r=[xbufs[ti]], w=[xt])
            rms_hT(kb, xt, gcol, hT, sqs, ps_ss, rs, consts, TT)
            for fb in range(22):
                pg, pu, sg = psg[fb % 2], psu[fb % 2], sgs[fb % 2]
                kb.op("pe", [w1, hT], [pg],
                      lambda e: [e.matmul(pg.t[:], w1.t[:, c, fb * 128:(fb + 1) * 128], hT.t[:, c, :],
                                          start=(c == 0), stop=(c == 7)) for c in range(8)][-1])
                kb.op("pe", [w1, hT], [pu],
                      lambda e: [e.matmul(pu.t[:], w1.t[:, c, DFF + fb * 128:DFF + (fb + 1) * 128], hT.t[:, c, :],
                                          start=(c == 0), stop=(c == 7)) for c in range(8)][-1])
                kb.op("act", [pg], [sg], lambda e: e.activation(out=sg.t[:], in_=pg.t[:], func=AF.Silu))
                kb.op("dve", [sg, pu], [aT],
                      lambda e: e.tensor_tensor(out=aT.t[:, fb, :], in0=sg.t[:], in1=pu.t[:], op=ALU.mult))
            for db in range(8):
                po = pso[db % 2]
                kb.op("pe", [w2, aT], [po],
                      lambda e: [e.matmul(po.t[:], w2.t[:, fc, db * 128:(db + 1) * 128], aT.t[:, fc, :],
                                          start=(fc == 0), stop=(fc == 21)) for fc in range(22)][-1])
                kb.op("dve", [po, xt], [xt],
                      lambda e: e.scalar_tensor_tensor(out=xt.t[:, db, :], in0=po.t[:], scalar=0.5, in1=xt.t[:, db, :],
                                                       op0=ALU.mult, op1=ALU.add))
            kb.dma("sp", xT_d[:, :, sl], xt.t[:, :, :], r=[xt], w=[xbufs[ti]])
    kb.barrier()

import os
STOPAT = int(os.environ.get("STOPAT", "99"))

OFF_RET = 0


def load_cols_bf16(kb, dst, w_d, col0, ncols):
    for c in range(8):
        kb.dma("pool", dst.t[:, c, :], w_d[c * 128:(c + 1) * 128, col0:col0 + ncols], r=[], w=[dst])


def proj_fm(kb, ps, w, wcol0, M, hT, tok0, W, out_p0=0):
    kb.op("pe", [w, hT], [ps],
          lambda e: [e.matmul(ps.t[out_p0:out_p0 + M, :W], w.t[:, c, wcol0:wcol0 + M], hT.t[:, c, tok0:tok0 + W],
                              start=(c == 0), stop=(c == 7)) for c in range(8)][-1])


def rope_fm(kb, ps, raw, psr, rot, cs, tok0, W, t1, t2, dst, dcol0, P=128):
    kb.op("act", [ps], [raw], lambda e: e.activation(out=raw.t[:P, :W], in_=ps.t[:P, :W], func=AF.Copy))
    kb.op("pe", [rot, raw], [psr], lambda e: e.matmul(psr.t[:P, :W], rot.t[:P, :P], raw.t[:P, :W], start=True, stop=True))
    kb.op("dve", [raw, cs], [t1],
          lambda e: e.tensor_tensor(out=t1.t[:P, :W], in0=raw.t[:P, :W], in1=cs.t[:P, 0, tok0:tok0 + W], op=ALU.mult))
    kb.op("dve", [psr, cs], [t2],
          lambda e: e.tensor_tensor(out=t2.t[:P, :W], in0=psr.t[:P, :W], in1=cs.t[:P, 1, tok0:tok0 + W], op=ALU.mult))
    kb.op("pool", [t1, t2], [dst],
          lambda e: e.tensor_tensor(out=dst.t[:P, dcol0:dcol0 + W], in0=t1.t[:P, :W], in1=t2.t[:P, :W], op=ALU.add))


def stage_retention(kb, hT, T, seq_off, w_d, yT_d, ybuf, C, G):
    nc = kb.nc
    NCH = T // 128
    NTT = T // 512
    with contextlib.ExitStack() as es:
        wq = kb.sb(es, [128, 8, 512], BF16, "wq")
        wk = kb.sb(es, [128, 8, 512], BF16, "wk")
        wv = kb.sb(es, [128, 8, 1024], BF16, "wv")
        wg = kb.sb(es, [128, 8, 1024], BF16, "wg")
        load_cols_bf16(kb, wv, w_d, OFF_RET + 1024, 1024)
        load_cols_bf16(kb, wq, w_d, OFF_RET + 0, 512)
        load_cols_bf16(kb, wk, w_d, OFF_RET + 512, 512)
        load_cols_bf16(kb, wg, w_d, OFF_RET + 2048, 1024)
        cs = kb.sb(es, [128, 2, T], F32, "cs")
        kb.dma("sp", cs.t[:, :, :], C["c_cs_ret"].rearrange("a p t -> p a t")[:, :, 0:T], r=[], w=[cs])
        rot = kb.sb(es, [128, 128], BF16, "rot")
        kb.dma("pool", rot.t[:, :], C["c_rot_ret"], r=[], w=[rot])
        maskT = kb.sb(es, [128, 8, 128], F32, "maskT")
        kb.dma("sp", maskT.t[:, :, :], C["c_ret_maskT"], r=[], w=[maskT])
        qdec = kb.sb(es, [128, 4, 128], F32, "qdec")
        kb.dma("sp", qdec.t[:, :, :], C["c_ret_qdec"], r=[], w=[qdec])
        kdec = kb.sb(es, [128, 8], F32, "kdec")
        kb.dma("sp", kdec.t[:, :], C["c_ret_kdec"], r=[], w=[kdec])
        v_sb = kb.sb(es, [128, NCH, 1024], BF16, "v_sb")
        qr = kb.sb(es, [128, T], BF16, "qr")
        kr = kb.sb(es, [128, T], BF16, "kr")
        qd = kb.sb(es, [128, T], BF16, "qd")
        sg = kb.sb(es, [128, T], BF16, "sg")
        o_sb = kb.sb(es, [128, T], F32, "o_sb")
        raw = kb.sb(es, [128, 512], BF16, "raw")
        t1 = kb.sb(es, [128, 512], F32, "t1")
        t2 = kb.sb(es, [128, 512], F32, "t2")
        sTm = [kb.sb(es, [128, 128], BF16, "sTm") for _ in range(2)]
        kd = kb.sb(es, [128, 64], BF16, "kd")
        state = kb.sb(es, [128, 128], F32, "state")
        state_bf = kb.sb(es, [128, 128], BF16, "state_bf")
        sq = kb.sb(es, [128, 512], F32, "sq")
        rs = kb.sb(es, [128, 512], F32, "rs")
        yo = [kb.sb(es, [128, 512], BF16, "yo") for _ in range(2)]
        pproj = [kb.ps(es, [128, 512], F32, "pproj") for _ in range(2)]
        psr = kb.ps(es, [128, 512], F32, "psr")
        bankS = kb.ps(es, [128, 512], F32, "bankS")
        psS = [View(bankS.t[:, k * 128:(k + 1) * 128], f"psS{k}", buf=bankS.b) for k in range(2)]
        psO = [kb.ps(es, [128, 512], F32, "psO") for _ in range(2)]
        psT = kb.ps(es, [128, 1024], BF16, "psT")
        psU = kb.ps(es, [128, 512], F32, "psU")
        ident = G["ident_bf"]
        ones_f = G["ones_f"]
        np_ = [0]

        def nextp():
            np_[0] += 1
            return pproj[np_[0] % 2]

        for tb in range(NCH):
            for half in range(2):
                ps = nextp()
                kb.op("pe", [wv, hT], [ps],
                      lambda e: [e.matmul(ps.t[:, :], hT.t[:, c, tb * 128:(tb + 1) * 128],
                                          wv.t[:, c, half * 512:(half + 1) * 512], start=(c == 0), stop=(c == 7))
                                 for c in range(8)][-1])
                kb.op("act", [ps], [v_sb],
                      lambda e: e.activation(out=v_sb.t[:, tb, half * 512:(half + 1) * 512], in_=ps.t[:, :], func=AF.Copy))

        if STOPAT <= 1:
            kb.barrier(); return
        for hp in range(4):
            for (w, dst) in ((wq, qr), (wk, kr)):
                for tt in range(NTT):
                    ps = nextp()
                    proj_fm(kb, ps, w, hp * 128, 128, hT, tt * 512, 512)
                    rope_fm(kb, ps, raw, psr, rot, cs, tt * 512, 512, t1, t2, dst, tt * 512)
            if STOPAT <= 2:
                kb.barrier(); return
            kb.op("pool", [qr, qdec], [qd],
                  lambda e: e.tensor_tensor(out=qd.t[:, :].rearrange("p (c i) -> p c i", i=128),
                                            in0=qr.t[:, :].rearrange("p (c i) -> p c i", i=128),
                                            in1=qdec.t[:, hp:hp + 1, :].to_broadcast([128, NCH, 128]), op=ALU.mult))
            if STOPAT <= 3:
                kb.barrier(); return
            for hh in range(2):
                h = 2 * hp + hh
                pb = 64 * hh
                for tt in range(NTT):
                    ps = nextp()
                    proj_fm(kb, ps, wg, h * 128, 128, hT, tt * 512, 512)
                    kb.op("act", [ps], [sg],
                          lambda e: e.activation(out=sg.t[:, tt * 512:(tt + 1) * 512], in_=ps.t[:, :], func=AF.Silu))
                for i in range(NCH):
                    cols = slice(i * 128, (i + 1) * 128)
                    pS, pO, sm = psS[i % 2], psO[i % 2], sTm[i % 2]
                    kb.op("pe", [kr, qr], [pS],
                          lambda e: e.matmul(pS.t[:, :], kr.t[pb:pb + 64, cols], qr.t[pb:pb + 64, cols], start=True, stop=True))
                    kb.op("dve", [pS, maskT], [sm],
                          lambda e: e.tensor_tensor(out=sm.t[:, :], in0=pS.t[:, :], in1=maskT.t[:, h, :], op=ALU.mult))
                    kb.op("pe", [v_sb, sm], [pO],
                          lambda e: e.matmul(pO.t[:, 0:128], v_sb.t[:, i, h * 128:(h + 1) * 128], sm.t[:, :],
                                             start=True, stop=(i == 0)))
                    if i > 0:
                        kb.op("pe", [state_bf, qd], [pO],
                              lambda e: e.matmul(pO.t[:, 0:128], state_bf.t[pb:pb + 64, :], qd.t[pb:pb + 64, cols],
                                                 start=False, stop=True))
                    kb.op("act", [pO], [o_sb], lambda e: e.activation(out=o_sb.t[:, cols], in_=pO.t[:, 0:128], func=AF.Copy))
                    if STOPAT <= 4:
                        kb.barrier(); return
                    if i < NCH - 1:
                        kb.op("pe", [kr, ident], [psT],
                              lambda e: e.transpose(psT.t[:, 0:64], kr.t[pb:pb + 64, cols], ident.t[pb:pb + 64, pb:pb + 64]))
                        kb.op("act", [psT, kdec], [kd],
                              lambda e: e.activation(out=kd.t[:, :], in_=psT.t[:, 0:64], func=AF.Copy, scale=kdec.t[:, h:h + 1]))
                        kb.op("pe", [kd, v_sb], [psU],
                              lambda e: e.matmul(psU.t[pb:pb + 64, 0:128], kd.t[:, :], v_sb.t[:, i, h * 128:(h + 1) * 128],
                                                 start=True, stop=True))
                        if i == 0:
                            kb.op("dve", [psU], [state],
                                  lambda e: e.tensor_copy(out=state.t[pb:pb + 64, :], in_=psU.t[pb:pb + 64, 0:128]))
                        else:
                            kb.op("dve", [psU, state], [state],
                                  lambda e: e.scalar_tensor_tensor(out=state.t[pb:pb + 64, :], in0=state.t[pb:pb + 64, :],
                                                                   scalar=C["ret_cd"][h], in1=psU.t[pb:pb + 64, 0:128],
                                                                   op0=ALU.mult, op1=ALU.add))
                        kb.op("act", [state], [state_bf],
                              lambda e: e.activation(out=state_bf.t[pb:pb + 64, :], in_=state.t[pb:pb + 64, :], func=AF.Copy))
                if STOPAT <= 5:
                    kb.barrier(); return
                for tt in range(NTT):
                    tc_ = slice(tt * 512, (tt + 1) * 512)
                    ps = nextp()
                    y = yo[tt % 2]
                    kb.op("act", [o_sb], [sq], lambda e: e.activation(out=sq.t[:, :], in_=o_sb.t[:, tc_], func=AF.Square))
                    kb.op("pe", [sq, ones_f], [ps], lambda e: e.matmul(ps.t[:, :], ones_f.t[:, :], sq.t[:, :], start=True, stop=True))
                    kb.op("act", [ps], [rs],
                          lambda e: e.activation(out=rs.t[:, :], in_=ps.t[:, :], func=AF.Sqrt, scale=1.0 / 128, bias=eps_ap(kb)))
                    kb.op("dve", [rs], [rs], lambda e: e.reciprocal(out=rs.t[:, :], in_=rs.t[:, :]))
                    kb.op("dve", [rs, o_sb], [rs],
                          lambda e: e.tensor_tensor(out=rs.t[:, :], in0=rs.t[:, :], in1=o_sb.t[:, tc_], op=ALU.mult))
                    kb.op("dve", [rs, sg], [y],
                          lambda e: e.tensor_tensor(out=y.t[:, :], in0=rs.t[:, :], in1=sg.t[:, tc_], op=ALU.mult))
                    kb.dma("sp", yT_d[h * 128:(h + 1) * 128, seq_off + tt * 512: seq_off + (tt + 1) * 512], y.t[:, :],
                           r=[y], w=[ybuf])
    kb.barrier()


import os


def run_lockstep(gens):
    gens = list(gens)
    while gens:
        nxt = []
        for g in gens:
            try:
                next(g)
                nxt.append(g)
            except StopIteration:
                pass
        gens = nxt

OFF_DSA = 3072
NEG_FILL = -3.0e38
NIT = 22
SGN_SCALE = float(2 ** 20)
EXACT_QB = 1


def rope_fm2(kb, ps, raw, psr, rot, cs, tok0, W, t1, t2, dst, dst_ap):
    kb.op("act", [ps], [raw], lambda e: e.activation(out=raw.t[:, :W], in_=ps.t[:, :W], func=AF.Copy))
    kb.op("pe", [rot, raw], [psr], lambda e: e.matmul(psr.t[:, :W], rot.t[:, :], raw.t[:, :W], start=True, stop=True))
    kb.op("dve", [raw, cs], [t1],
          lambda e: e.tensor_tensor(out=t1.t[:, :W], in0=raw.t[:, :W], in1=cs.t[:, 0, tok0:tok0 + W], op=ALU.mult))
    kb.op("dve", [psr, cs], [t2],
          lambda e: e.tensor_tensor(out=t2.t[:, :W], in0=psr.t[:, :W], in1=cs.t[:, 1, tok0:tok0 + W], op=ALU.mult))
    kb.op("pool", [t1, t2], [dst],
          lambda e: e.tensor_tensor(out=dst_ap, in0=t1.t[:, :W], in1=t2.t[:, :W], op=ALU.add))


def stage_dsa(kb, hT, T, seq_off, w_d, yT_d, ybuf, C, G, ktop=256):
    NQB = T // 128
    NTT = T // 512
    ident = G["ident_bf"]
    with contextlib.ExitStack() as es:
        negmask = kb.sb(es, [128, 128], F32, "negmask")
        kb.dma("sp", negmask.t[:, :], C["c_negmask"], r=[], w=[negmask])
        qr = kb.sb(es, [128, 4, T], BF16, "qr")
        qir = kb.sb(es, [128, 2, T], BF16, "qir")
        kr2 = kb.sb(es, [128, T], BF16, "kr2")
        kir2 = kb.sb(es, [128, T], BF16, "kir2")
        v_sb = kb.sb(es, [128, NQB, 64], BF16, "v_sb")
        ones64 = kb.sb(es, [128, 64], BF16, "ones64")
        kb.op("dve", [], [ones64], lambda e: e.memset(ones64.t[:, :], 1.0))
        wabs = kb.sb(es, [128, NQB, 4], F32, "wabs")
        wsgn = kb.sb(es, [128, NQB, 4], F32, "wsgn")

        with contextlib.ExitStack() as ea:
            wd = kb.sb(ea, [128, 8, 964], BF16, "wd")
            load_cols_bf16(kb, wd, w_d, OFF_DSA, 964)
            cs = kb.sb(ea, [128, 2, T], F32, "cs")
            kb.dma("sp", cs.t[:, :, :], C["c_cs_dsa"].rearrange("a p t -> p a t")[:, :, 0:T], r=[], w=[cs])
            rot = kb.sb(ea, [128, 128], BF16, "rot")
            kb.dma("pool", rot.t[:, :], C["c_rot_dsa"], r=[], w=[rot])
            pproj = [kb.ps(ea, [128, 512], F32, "pproj") for _ in range(2)]
            psr = kb.ps(ea, [128, 512], F32, "psr")
            raw = kb.sb(ea, [128, 512], BF16, "raw")
            t1 = kb.sb(ea, [128, 512], F32, "t1")
            t2 = kb.sb(ea, [128, 512], F32, "t2")
            n_ = [0]

            def nextp():
                n_[0] += 1
                return pproj[n_[0] % 2]

            for tt in range(NTT):
                tok0 = tt * 512
                for j in range(4):
                    ps = nextp()
                    proj_fm(kb, ps, wd, j * 128, 128, hT, tok0, 512)
                    rope_fm2(kb, ps, raw, psr, rot, cs, tok0, 512, t1, t2, qr, qr.t[:, j, tok0:tok0 + 512])
                for j in range(2):
                    ps = nextp()
                    proj_fm(kb, ps, wd, 640 + j * 128, 128, hT, tok0, 512)
                    rope_fm2(kb, ps, raw, psr, rot, cs, tok0, 512, t1, t2, qir, qir.t[:, j, tok0:tok0 + 512])
                for (col, dst) in ((512, kr2), (896, kir2)):
                    ps = nextp()
                    proj_fm(kb, ps, wd, col, 64, hT, tok0, 512, out_p0=0)
                    proj_fm(kb, ps, wd, col, 64, hT, tok0, 512, out_p0=64)
                    rope_fm2(kb, ps, raw, psr, rot, cs, tok0, 512, t1, t2, dst, dst.t[:, tok0:tok0 + 512])
            for qb in range(NQB):
                ps = nextp()
                qc = slice(qb * 128, (qb + 1) * 128)
                for c in range(8):
                    kb.op("pe", [wd, hT], [ps],
                          lambda e: e.matmul(ps.t[:, 0:64], hT.t[:, c, qc], wd.t[:, c, 576:640], start=(c == 0), stop=(c == 7)))
                for c in range(8):
                    kb.op("pe", [wd, hT], [ps],
                          lambda e: e.matmul(ps.t[:, 64:68], hT.t[:, c, qc], wd.t[:, c, 960:964], start=(c == 0), stop=(c == 7)))
                kb.op("act", [ps], [v_sb], lambda e: e.activation(out=v_sb.t[:, qb, :], in_=ps.t[:, 0:64], func=AF.Copy))
                kb.op("act", [ps], [wabs],
                      lambda e: e.activation(out=wabs.t[:, qb, :], in_=ps.t[:, 64:68], func=AF.Abs, scale=0.5 / 8.0))
                kb.op("act", [ps], [wsgn], lambda e: e.activation(out=wsgn.t[:, qb, :], in_=ps.t[:, 64:68], func=AF.Sign))
        kb.barrier()

        with contextlib.ExitStack() as eb:
            NC3 = 3
            psI = [kb.ps(eb, [128, 512], F32, "psI") for _ in range(2)]
            psMT = kb.ps(eb, [128, 1024], BF16, "psMT")
            psS = [kb.ps(eb, [128, 512], F32, "psS") for _ in range(2)]
            psO = kb.ps(eb, [128, 512], F32, "psO")
            psD = kb.ps(eb, [128, 512], F32, "psD")
            scr = kb.sb(eb, [128, T], BF16, "scr")
            onesT = kb.sb(eb, [128, T], BF16, "onesT")
            kb.op("dve", [], [onesT], lambda e: e.memset(onesT.t[:, :], 1.0))
            pw2 = kb.sb(eb, [128, NIT + 1], F32, "pw2")
            for k in range(NIT + 1):
                kb.op("dve", [], [pw2], lambda e: e.memset(pw2.t[:, k:k + 1], 2.0 ** (-k)))
            pT = [kb.sb(eb, [128, 512], BF16, "pT") for _ in range(2)]
            rD = kb.sb(eb, [128, 512], F32, "rD")
            yb = [kb.sb(eb, [128, 512], BF16, "yb") for _ in range(2)]
            ctxs = []
            for g in range(NC3):
                cx = {}
                cx["sc"] = kb.sb(eb, [128, T], F32, "sc")
                cx["junk"] = kb.sb(eb, [128, T], BF16, "junk")
                cx["csum"] = kb.sb(eb, [128, T], F32, "csum")
                cx["mask"] = kb.sb(eb, [128, T], BF16, "mask")
                cx["maskT"] = kb.sb(eb, [128, NQB, 128], BF16, "maskT")
                cx["rl"] = [kb.sb(eb, [128, 512], F32, "rl") for _ in range(2)]
                for nm, w_ in (("cnt", 1), ("ge", 1), ("lo", 1), ("negmid", 1), ("amax", 1), ("gpos", 1), ("halfs", NIT + 1)):
                    cx[nm] = kb.sb(eb, [128, w_], F32, nm)
                ctxs.append(cx)
            itc = [0]

            def sel_gen(qb, cx):
                sc, junk, csum, mask, maskT = cx["sc"], cx["junk"], cx["csum"], cx["mask"], cx["maskT"]
                cnt, ge, lo, negmid, amax, gpos, halfs = (cx[n] for n in ("cnt", "ge", "lo", "negmid", "amax", "gpos", "halfs"))
                nk = 128 * (qb + 1)
                qc = slice(qb * 128, (qb + 1) * 128)
                for kc in range((nk + 511) // 512):
                    kw = min(512, nk - kc * 512)
                    kcols = slice(kc * 512, kc * 512 + kw)
                    for h4 in range(4):
                        pair, pb = h4 // 2, 64 * (h4 % 2)
                        itc[0] += 1
                        pI, r_ = psI[itc[0] % 2], cx["rl"][h4 % 2]
                        kb.op("pe", [qir, kir2], [pI],
                              lambda e: e.matmul(pI.t[:, :kw], qir.t[pb:pb + 64, pair, qc], kir2.t[pb:pb + 64, kcols],
                                                 start=True, stop=True))
                        kb.op("act", [pI, wabs], [r_],
                              lambda e: e.activation(out=r_.t[:, :kw], in_=pI.t[:, :kw], func=AF.Relu,
                                                     scale=wabs.t[:, qb, h4:h4 + 1]))
                        if h4 == 0:
                            kb.op("dve", [r_, wsgn], [sc],
                                  lambda e: e.tensor_scalar(out=sc.t[:, kcols], in0=r_.t[:, :kw], scalar1=wsgn.t[:, qb, 0:1],
                                                            scalar2=None, op0=ALU.mult))
                        else:
                            kb.op("dve", [r_, wsgn, sc], [sc],
                                  lambda e: e.scalar_tensor_tensor(out=sc.t[:, kcols], in0=r_.t[:, :kw],
                                                                   scalar=wsgn.t[:, qb, h4:h4 + 1], in1=sc.t[:, kcols],
                                                                   op0=ALU.mult, op1=ALU.add))
                        yield
                if nk > ktop:
                    kb.op("dve", [sc], [amax],
                          lambda e: e.tensor_reduce(out=amax.t[:, :], in_=sc.t[:, :nk], axis=AX.X, op=ALU.max,
                                                    apply_absolute_value=True))
                    kb.op("dve", [amax], [amax],
                          lambda e: e.tensor_scalar(out=amax.t[:, :], in0=amax.t[:, :], scalar1=1.0, scalar2=None, op0=ALU.add))
                    kb.op("dve", [amax, pw2], [halfs],
                          lambda e: e.tensor_scalar(out=halfs.t[:, :], in0=pw2.t[:, :], scalar1=amax.t[:, 0:1], scalar2=1.25,
                                                    op0=ALU.mult, op1=ALU.mult))
                    kb.op("dve", [amax], [lo],
                          lambda e: e.tensor_scalar(out=lo.t[:, :], in0=amax.t[:, :], scalar1=-1.5, scalar2=None, op0=ALU.mult))
                    kb.op("dve", [lo, halfs], [negmid],
                          lambda e: e.tensor_scalar(out=negmid.t[:, :], in0=lo.t[:, :], scalar1=halfs.t[:, 0:1], scalar2=-SGN_SCALE,
                                                    op0=ALU.add, op1=ALU.mult))
                kb.op("dve", [sc, negmask], [sc],
                      lambda e: e.tensor_tensor(out=sc.t[:, qc], in0=sc.t[:, qc], in1=negmask.t[:, :], op=ALU.add))
                yield
                if nk > ktop:
                    for k in range(NIT):
                        kb.op("act", [sc, negmid], [junk, cnt],
                              lambda e: e.activation(out=junk.t[:, :nk], in_=sc.t[:, :nk], func=AF.Sign, bias=negmid.t[:, 0:1],
                                                     scale=SGN_SCALE, accum_out=cnt.t[:, 0:1]))
                        yield
                        kb.op("dve", [cnt], [ge],
                              lambda e: e.tensor_scalar(out=ge.t[:, :], in0=cnt.t[:, :], scalar1=float(2 * ktop - nk), scalar2=None,
                                                        op0=ALU.is_ge))
                        kb.op("dve", [ge, halfs, lo], [lo],
                              lambda e: e.scalar_tensor_tensor(out=lo.t[:, :], in0=ge.t[:, :], scalar=halfs.t[:, k:k + 1],
                                                               in1=lo.t[:, :], op0=ALU.mult, op1=ALU.add))
                        kb.op("dve", [lo, halfs], [negmid],
                              lambda e: e.tensor_scalar(out=negmid.t[:, :], in0=lo.t[:, :], scalar1=halfs.t[:, k + 1:k + 2],
                                                        scalar2=-SGN_SCALE, op0=ALU.add, op1=ALU.mult))
                        yield
                    kb.op("dve", [sc, lo], [mask],
                          lambda e: e.tensor_scalar(out=mask.t[:, :nk], in0=sc.t[:, :nk], scalar1=lo.t[:, 0:1], scalar2=None,
                                                    op0=ALU.is_gt))
                    yield
                    kb.op("dve", [sc], [junk],
                          lambda e: e.tensor_scalar(out=junk.t[:, :nk], in0=sc.t[:, :nk], scalar1=0.0, scalar2=None, op0=ALU.is_equal))
                    yield
                    kb.op("dve", [sc], [scr, gpos],
                          lambda e: e.tensor_scalar(out=scr.t[:, :nk], in0=sc.t[:, :nk], scalar1=0.0, scalar2=None, op0=ALU.is_gt,
                                                    op1=ALU.add, accum_out=gpos.t[:, 0:1]))
                    kb.op("dve", [gpos], [gpos],
                          lambda e: e.tensor_scalar(out=gpos.t[:, :], in0=gpos.t[:, :], scalar1=-1.0, scalar2=float(ktop),
                                                    op0=ALU.mult, op1=ALU.add))
                    kb.op("dve", [gpos], [gpos],
                          lambda e: e.tensor_scalar(out=gpos.t[:, :], in0=gpos.t[:, :], scalar1=0.0, scalar2=None, op0=ALU.max))
                    yield
                    kb.op("dve", [junk, onesT], [csum],
                          lambda e: e.tensor_tensor_scan(out=csum.t[:, :nk], data0=onesT.t[:, :nk], data1=junk.t[:, :nk], initial=0.0,
                                                         op0=ALU.mult, op1=ALU.add))
                    yield
                    kb.op("dve", [junk, csum], [csum],
                          lambda e: e.tensor_tensor(out=csum.t[:, :nk], in0=csum.t[:, :nk], in1=junk.t[:, :nk], op=ALU.mult))
                    yield
                    kb.op("dve", [csum, gpos, mask], [mask],
                          lambda e: e.scalar_tensor_tensor(out=mask.t[:, :nk], in0=csum.t[:, :nk], scalar=gpos.t[:, 0:1],
                                                           in1=mask.t[:, :nk], op0=ALU.is_le, op1=ALU.mult))
                else:
                    kb.op("dve", [sc], [mask],
                          lambda e: e.tensor_scalar(out=mask.t[:, :nk], in0=sc.t[:, :nk], scalar1=-1.0e29, scalar2=None,
                                                    op0=ALU.is_gt))
                yield
                for kb0 in range(0, qb + 1, 8):
                    nb = min(8, qb + 1 - kb0)
                    for k2 in range(nb):
                        kbi = kb0 + k2
                        kb.op("pe", [mask, ident], [psMT],
                              lambda e: e.transpose(psMT.t[:, k2 * 128:(k2 + 1) * 128], mask.t[:, kbi * 128:(kbi + 1) * 128],
                                                    ident.t[:, :]))
                    kb.op("act", [psMT], [maskT],
                          lambda e: e.activation(out=maskT.t[:, kb0:kb0 + nb, :].rearrange("p a b -> p (a b)"),
                                                 in_=psMT.t[:, 0:nb * 128], func=AF.Copy))
                    yield

            def attn(qb, cx):
                maskT = cx["maskT"]
                qc = slice(qb * 128, (qb + 1) * 128)
                for par in range(2):
                    pb = 64 * par
                    for kbi in range(qb + 1):
                        itc[0] += 1
                        pS, p_ = psS[itc[0] % 2], pT[itc[0] % 2]
                        kc_ = slice(kbi * 128, (kbi + 1) * 128)
                        kb.op("pe", [kr2, qr], [pS],
                              lambda e: e.matmul(pS.t[:, :].rearrange("p (a b) -> p a b", b=128), kr2.t[pb:pb + 64, kc_],
                                                 qr.t[pb:pb + 64, :, qc], start=True, stop=True))
                        kb.op("act", [pS], [p_], lambda e: e.activation(out=p_.t[:, :], in_=pS.t[:, :], func=AF.Exp, scale=0.125))
                        kb.op("dve", [p_, maskT], [p_],
                              lambda e: e.tensor_tensor(out=p_.t[:, :].rearrange("p (a b) -> p a b", b=128),
                                                        in0=p_.t[:, :].rearrange("p (a b) -> p a b", b=128),
                                                        in1=maskT.t[:, kbi:kbi + 1, :].to_broadcast([128, 4, 128]), op=ALU.mult))
                        kb.op("pe", [v_sb, p_], [psO],
                              lambda e: e.matmul(psO.t[0:64, :], v_sb.t[:, kbi, :], p_.t[:, :], start=(kbi == 0), stop=(kbi == qb)))
                        kb.op("pe", [ones64, p_], [psD],
                              lambda e: e.matmul(psD.t[0:64, :], ones64.t[:, :], p_.t[:, :], start=(kbi == 0), stop=(kbi == qb)))
                    y_ = yb[par]
                    kb.op("dve", [psD], [rD], lambda e: e.reciprocal(out=rD.t[0:64, :], in_=psD.t[0:64, :]))
                    kb.op("dve", [psO, rD], [y_],
                          lambda e: e.tensor_tensor(out=y_.t[0:64, :], in0=psO.t[0:64, :], in1=rD.t[0:64, :], op=ALU.mult))
                    dst = yT_d[1024:1536, seq_off + qb * 128: seq_off + (qb + 1) * 128].rearrange(
                        "(j two d) t -> two d j t", two=2, d=64)[par]
                    kb.dma("sp", dst, y_.t[0:64, :].rearrange("p (a b) -> p a b", b=128), r=[y_], w=[ybuf])

            for g0 in range(0, NQB, NC3):
                grp = list(range(g0, min(NQB, g0 + NC3)))
                run_lockstep([sel_gen(qb, ctxs[i]) for i, qb in enumerate(grp)])
                for i, qb in enumerate(grp):
                    attn(qb, ctxs[i])
    kb.barrier()

import math, os
STOPAT = int(os.environ.get('STOPAT', '99'))

OFF_RWKV = 4036
POOLENG = os.environ.get("POOLENG", "dve")
EM05 = math.exp(-0.5)
GN_EPS = 64e-5


def run_lockstep(gens):
    gens = list(gens)
    while gens:
        nxt = []
        for g in gens:
            try:
                next(g)
                nxt.append(g)
            except StopIteration:
                pass
        gens = nxt


def stage_rwkv(kb, hT, T, seq_off, w_d, PL, yT_d, ybuf, C, G):
    NTT = T // 512
    NB = T // 128
    NCK = T // 64
    ident_bf = G["ident_bf"]
    ident_f = G["ident_f"]
    with contextlib.ExitStack() as es:
        colp = kb.sb(es, [128, 4, 8], F32, "colp")
        for idx, nm in enumerate(["rwkv_w0", "rwkv_a0", "rwkv_k_k", "rwkv_k_a", "rwkv_r_k", "rwkv_ln_w", "rwkv_ln_b"]):
            kb.dma("sp", colp.t[:, :, idx], PL[nm].rearrange("(j p) -> p j", p=128), r=[], w=[colp],
                   allow_slow_non_contiguous=True)
        kb.op("dve", [colp], [colp],
              lambda e: e.tensor_scalar(out=colp.t[:, :, 7], in0=colp.t[:, :, 3], scalar1=-1.0, scalar2=1.0, op0=ALU.mult, op1=ALU.add))
        mu = kb.sb(es, [128, 14, 2], F32, "mu")
        kb.dma("sp", mu.t[:, :, 0], PL["rwkv_mu"].rearrange("(f p) -> p f", p=128), r=[], w=[mu], allow_slow_non_contiguous=True)
        kb.op("dve", [mu], [mu],
              lambda e: e.tensor_scalar(out=mu.t[:, :, 1], in0=mu.t[:, :, 0], scalar1=-1.0, scalar2=1.0, op0=ALU.mult, op1=ALU.add))
        w2a2 = kb.sb(es, [128, 512], BF16, "w2a2")
        kb.dma("pool", w2a2.t[0:64, :], PL["rwkv_w2"], r=[], w=[w2a2])
        kb.dma("pool", w2a2.t[64:128, :], PL["rwkv_a2"], r=[], w=[w2a2])
        g2b = kb.sb(es, [128, 512], BF16, "g2b")
        kb.dma("pool", g2b.t[:, :], PL["rwkv_g2"], r=[], w=[g2b])
        blk1 = kb.sb(es, [128, 128], F32, "blk1")
        kb.dma("sp", blk1.t[:, :], C["c_blockones"], r=[], w=[blk1])
        mN = kb.sb(es, [128, 128], F32, "mN")
        mNT = kb.sb(es, [128, 128], F32, "mNT")
        mIT = kb.sb(es, [128, 128], F32, "mIT")
        kb.dma("sp", mN.t[:, :], C["c_maskN"], r=[], w=[mN])
        kb.dma("sp", mNT.t[:, :], C["c_maskNT"], r=[], w=[mNT])
        kb.dma("sp", mIT.t[:, :], C["c_maskIT"], r=[], w=[mIT])
        resetm = kb.sb(es, [128, T], BF16, "resetm")
        kb.op("dve", [], [resetm], lambda e: e.memset(resetm.t[:, :], 1.0))
        kb.op("dve", [resetm], [resetm],
              lambda e: e.memset(resetm.t[:, :].rearrange("p (c i) -> p c i", i=64)[:, :, 0:1], 0.0))
        wl = kb.sb(es, [128, 8, 256], BF16, "wl")
        for c in range(8):
            kb.dma("pool", wl.t[:, c, :], w_d[c * 128:(c + 1) * 128, OFF_RWKV + 1536:OFF_RWKV + 1792], r=[], w=[wl])
        wp = [kb.sb(es, [128, 8, 384], BF16, "wp")]
        lo = kb.sb(es, [128, T], BF16, "lo")
        sgl = kb.sb(es, [128, T], BF16, "sgl")
        S = [kb.sb(es, [128, T + (1 if i == 7 else 0)], F32, f"S{i}") if i != 2 else None for i in range(8)]
        g_bf = kb.sb(es, [128, T], BF16, "g_bf")
        At = kb.sb(es, [128, T], BF16, "At")
        Bt = kb.sb(es, [128, T], BF16, "Bt")
        BtP = kb.sb(es, [128, T], BF16, "BtP")
        Kt = kb.sb(es, [128, T], BF16, "Kt")
        KtP = kb.sb(es, [128, T], BF16, "KtP")
        Rt = kb.sb(es, [128, T], BF16, "Rt")
        vb = kb.sb(es, [128, T], BF16, "vb")
        tok = kb.sb(es, [128, NB, 512], BF16, "tok")
        PCc = kb.sb(es, [128, NCK], F32, "PCc")
        ApT_all = kb.sb(es, [128, NB, 128], BF16, "ApT_all")
        W_all = kb.sb(es, [128, NB, 2, 64], F32, "W_all")
        ArbT_all = kb.sb(es, [128, NB, 2, 128], BF16, "ArbT_all")
        ArkT_all = kb.sb(es, [128, NB, 2, 128], BF16, "ArkT_all")
        Hf = kb.sb(es, [128, 64], F32, "Hf")
        Hb_all = kb.sb(es, [128, NCK + 1, 64], BF16, "Hb_all")
        rn = kb.sb(es, [128, 512], F32, "rn")
        yo = [kb.sb(es, [128, 512], BF16, "yo") for _ in range(2)]

        def shift_block(ps_list, fb, PA, PB, dst_fn):
            pass

        BK = [kb.ps(es, [128, 512], F32, "bk") for _ in range(7)]
        with contextlib.ExitStack() as ea:
            pproj = BK[0:2]
            psT = kb.ps(ea, [128, 1024], BF16, "psT")
            n_ = [0]

            def nextp():
                n_[0] += 1
                return pproj[n_[0] % 2]

            x_ = [0]

            nextx = nextp

            PA, PB = S[6], S[7]
            kb.op("dve", [], [PB], lambda e: e.memset(PB.t[:, 0:1], 0.0))

            def shifted(w, wcol0, fb, dst):
                kb.op("dve", [], [PB], lambda e: e.memset(PB.t[:, 0:1], 0.0))
                for tt in range(NTT):
                    ps = nextp()
                    tc_ = slice(tt * 512, (tt + 1) * 512)
                    proj_fm(kb, ps, w, wcol0, 128, hT, tt * 512, 512)
                    kb.op("act", [ps, mu], [PA],
                          lambda e: e.activation(out=PA.t[:, tc_], in_=ps.t[:, :], func=AF.Copy, scale=mu.t[:, fb, 1:2]))
                    kb.op("dve", [ps, mu], [PB],
                          lambda e: e.tensor_scalar(out=PB.t[:, 1 + tt * 512:1 + (tt + 1) * 512], in0=ps.t[:, :],
                                                    scalar1=mu.t[:, fb, 0:1], scalar2=None, op0=ALU.mult))
                kb.op(POOLENG, [PA, PB], [dst],
                      lambda e: e.tensor_tensor(out=dst.t[:, 0:T], in0=PA.t[:, 0:T], in1=PB.t[:, 0:T], op=ALU.add))

            if STOPAT <= 1:
                kb.barrier(); return
            shifted(wl, 0, 12, S[0])
            kb.op("act", [S[0]], [lo], lambda e: e.activation(out=lo.t[0:64, :], in_=S[0].t[0:64, 0:T], func=AF.Tanh))
            kb.op("act", [S[0]], [lo], lambda e: e.activation(out=lo.t[64:128, :], in_=S[0].t[64:128, 0:T], func=AF.Copy))
            shifted(wl, 128, 13, S[0])
            kb.op("act", [S[0]], [sgl], lambda e: e.activation(out=sgl.t[:, :], in_=S[0].t[:, 0:T], func=AF.Sigmoid))

            if os.environ.get("REPEAT"):
                shifted(wl, 128, 13, S[0])
            if STOPAT <= 2:
                kb.barrier(); return
            for j in range(4):
                pc = slice(j * 128, (j + 1) * 128)
                wpj = wp[0]
                for c in range(0 if os.environ.get("SKIPDMA") else 8):
                    kb.dma("pool", wpj.t[:, c, :].rearrange("p (k f) -> p k f", k=3),
                           w_d[c * 128:(c + 1) * 128, OFF_RWKV:OFF_RWKV + 1536].rearrange("p (k f) -> p k f", k=3)[:, :, j * 128:(j + 1) * 128],
                           r=[], w=[wpj])
                r_, k_, v_ = S[0], S[1], vb
                shifted(wpj, 0, j, r_)
                if os.environ.get("ONLYR"):
                    kb.barrier(); return
                shifted(wpj, 128, 4 + j, k_)
                if not os.environ.get("SKIPV"):
                    shifted(wpj, 256, 8 + j, vb)
                cp = lambda idx: colp.t[:, j, idx:idx + 1]
                lw, a_, cum = S[3], S[4], S[5]
                if os.environ.get("STOP25"):
                    kb.barrier(); return
                for tt in range(NTT):
                    tc_ = slice(tt * 512, (tt + 1) * 512)
                    ps = nextx()
                    kb.op("pe", [w2a2, lo], [ps],
                          lambda e: e.matmul(ps.t[:, :], w2a2.t[0:64, pc], lo.t[0:64, tc_], start=True, stop=True))
                    kb.op("act", [ps, colp], [lw],
                          lambda e: e.activation(out=lw.t[:, tc_], in_=ps.t[:, :], func=AF.Sigmoid, bias=cp(0)))
                    ps = nextx()
                    kb.op("pe", [w2a2, lo], [ps],
                          lambda e: e.matmul(ps.t[:, :], w2a2.t[64:128, pc], lo.t[64:128, tc_], start=True, stop=True))
                    kb.op("act", [ps, colp], [a_],
                          lambda e: e.activation(out=a_.t[:, tc_], in_=ps.t[:, :], func=AF.Sigmoid, bias=cp(1)))
                    ps = nextx()
                    kb.op("pe", [g2b, sgl], [ps],
                          lambda e: e.matmul(ps.t[:, :], g2b.t[:, pc], sgl.t[:, tc_], start=True, stop=True))
                    kb.op("act", [ps], [g_bf], lambda e: e.activation(out=g_bf.t[:, tc_], in_=ps.t[:, :], func=AF.Copy))
                kb.op("dve", [lw], [lw],
                      lambda e: e.tensor_scalar(out=lw.t[:, 0:T], in0=lw.t[:, 0:T], scalar1=-EM05, scalar2=None, op0=ALU.mult))
                if os.environ.get("SKIPSCAN"):
                    kb.barrier(); return
                kb.op("dve", [resetm, lw], [cum],
                      lambda e: e.tensor_tensor_scan(out=cum.t[:, 0:T], data0=resetm.t[:, :], data1=lw.t[:, 0:T], initial=0.0,
                                                     op0=ALU.mult, op1=ALU.add))
                if STOPAT <= 3:
                    kb.barrier(); return
                kb.op(POOLENG, [cum, lw], [S[6]],
                      lambda e: e.tensor_tensor(out=S[6].t[:, 0:T], in0=cum.t[:, 0:T], in1=lw.t[:, 0:T], op=ALU.subtract))
                kb.op("act", [S[6]], [S[6]], lambda e: e.activation(out=S[6].t[:, 0:T], in_=S[6].t[:, 0:T], func=AF.Exp))
                kk = S[7]
                for tt in range(NTT):
                    tc_ = slice(tt * 512, (tt + 1) * 512)
                    ps = nextx()
                    kb.op("act", [k_, colp], [kk],
                          lambda e: e.activation(out=kk.t[:, tc_], in_=k_.t[:, tc_], func=AF.Square, scale=cp(2)))
                    kb.op("pe", [blk1, kk], [ps], lambda e: e.matmul(ps.t[:, :], blk1.t[:, :], kk.t[:, tc_], start=True, stop=True))
                    kb.op("act", [ps], [rn], lambda e: e.activation(out=rn.t[:, :], in_=ps.t[:, :], func=AF.Sqrt))
                    kb.op("dve", [rn], [rn],
                          lambda e: e.tensor_scalar(out=rn.t[:, :], in0=rn.t[:, :], scalar1=1e-12, scalar2=None, op0=ALU.max))
                    kb.op("dve", [rn], [rn], lambda e: e.reciprocal(out=rn.t[:, :], in_=rn.t[:, :]))
                    kb.op("dve", [k_, colp, rn], [kk],
                          lambda e: e.scalar_tensor_tensor(out=kk.t[:, tc_], in0=k_.t[:, tc_], scalar=cp(2), in1=rn.t[:, :],
                                                           op0=ALU.mult, op1=ALU.mult))
                kb.op("dve", [kk, S[6]], [At],
                      lambda e: e.scalar_tensor_tensor(out=At.t[:, :], in0=kk.t[:, 0:T], scalar=-1.0, in1=S[6].t[:, 0:T],
                                                       op0=ALU.mult, op1=ALU.mult))
                kka = S[6]
                kb.op(POOLENG, [kk, a_], [kka],
                      lambda e: e.tensor_tensor(out=kka.t[:, 0:T], in0=kk.t[:, 0:T], in1=a_.t[:, 0:T], op=ALU.mult))
                kb.op("dve", [a_, colp], [a_],
                      lambda e: e.tensor_scalar(out=a_.t[:, 0:T], in0=a_.t[:, 0:T], scalar1=cp(3), scalar2=cp(7), op0=ALU.mult,
                                                op1=ALU.add))
                kh = a_
                kb.op(POOLENG, [k_, a_], [kh],
                      lambda e: e.tensor_tensor(out=kh.t[:, 0:T], in0=k_.t[:, 0:T], in1=a_.t[:, 0:T], op=ALU.mult))
                inv = S[1]
                kb.op("act", [cum], [inv], lambda e: e.activation(out=inv.t[:, 0:T], in_=cum.t[:, 0:T], func=AF.Exp, scale=-1.0))
                kb.op("dve", [kka, inv], [Bt],
                      lambda e: e.tensor_tensor(out=Bt.t[:, :], in0=kka.t[:, 0:T], in1=inv.t[:, 0:T], op=ALU.mult))
                kb.op("dve", [kh, inv], [Kt],
                      lambda e: e.tensor_tensor(out=Kt.t[:, :], in0=kh.t[:, 0:T], in1=inv.t[:, 0:T], op=ALU.mult))
                E = S[1]
                cum3 = cum.t[:, 0:T].rearrange("p (c i) -> p c i", i=64)
                kb.op("dve", [cum], [E],
                      lambda e: e.tensor_tensor(out=E.t[:, 0:T].rearrange("p (c i) -> p c i", i=64),
                                                in0=cum3[:, :, 63:64].to_broadcast([128, NCK, 64]), in1=cum3, op=ALU.subtract))
                kb.op("act", [E], [E], lambda e: e.activation(out=E.t[:, 0:T], in_=E.t[:, 0:T], func=AF.Exp))
                kb.op("dve", [kka, E], [BtP],
                      lambda e: e.tensor_tensor(out=BtP.t[:, :], in0=kka.t[:, 0:T], in1=E.t[:, 0:T], op=ALU.mult))
                kb.op("dve", [kh, E], [KtP],
                      lambda e: e.tensor_tensor(out=KtP.t[:, :], in0=kh.t[:, 0:T], in1=E.t[:, 0:T], op=ALU.mult))
                Pinc = S[3]
                kb.op("act", [cum], [Pinc], lambda e: e.activation(out=Pinc.t[:, 0:T], in_=cum.t[:, 0:T], func=AF.Exp))
                kb.op("dve", [r_, Pinc], [Rt],
                      lambda e: e.tensor_tensor(out=Rt.t[:, :], in0=r_.t[:, 0:T], in1=Pinc.t[:, 0:T], op=ALU.mult))
                kb.op("dve", [Pinc], [PCc],
                      lambda e: e.tensor_copy(out=PCc.t[:, :], in_=Pinc.t[:, 0:T].rearrange("p (c i) -> p c i", i=64)[:, :, 63]))
                rk = S[1]
                kb.op("dve", [r_, colp, kh], [rk],
                      lambda e: e.scalar_tensor_tensor(out=rk.t[:, 0:T], in0=r_.t[:, 0:T], scalar=cp(4), in1=kh.t[:, 0:T],
                                                       op0=ALU.mult, op1=ALU.mult))
                bon = S[6]
                for tt in range(NTT):
                    tc_ = slice(tt * 512, (tt + 1) * 512)
                    ps = nextx()
                    kb.op("pe", [blk1, rk], [ps], lambda e: e.matmul(ps.t[:, :], blk1.t[:, :], rk.t[:, tc_], start=True, stop=True))
                    kb.op("dve", [ps, v_], [bon],
                          lambda e: e.tensor_tensor(out=bon.t[:, tc_], in0=ps.t[:, :], in1=v_.t[:, tc_], op=ALU.mult))
                if STOPAT <= 4:
                    kb.barrier(); return
                for tb in range(NB):
                    cols = slice(tb * 128, (tb + 1) * 128)
                    for q4, src in enumerate((At, BtP, KtP, vb)):
                        kb.op("pe", [src, ident_bf], [psT],
                              lambda e: e.transpose(psT.t[:, q4 * 128:(q4 + 1) * 128], src.t[:, cols], ident_bf.t[:, :]))
                    kb.op("act", [psT], [tok], lambda e: e.activation(out=tok.t[:, tb, :], in_=psT.t[:, 0:512], func=AF.Copy))
                if STOPAT <= 5:
                    kb.barrier(); return
                yield_pair(kb, j, T, NB, NCK, At, Bt, Kt, Rt, tok, PCc, ApT_all, W_all, ArbT_all, ArkT_all, Hf, Hb_all,
                           mN, mNT, mIT, ident_f, S[0], BK, [S[1], S[3], S[4], S[5]], [BtP, vb], KtP)
                if STOPAT <= 7:
                    kb.barrier(); return
                y_sb = S[0]
                d_ = S[1]
                for tt in range(NTT):
                    tc_ = slice(tt * 512, (tt + 1) * 512)
                    ps = nextx()
                    kb.op("pe", [blk1, y_sb], [ps], lambda e: e.matmul(ps.t[:, :], blk1.t[:, :], y_sb.t[:, tc_], start=True, stop=True))
                    kb.op("dve", [ps, y_sb], [d_],
                          lambda e: e.scalar_tensor_tensor(out=d_.t[:, tc_], in0=ps.t[:, :], scalar=-1.0 / 64, in1=y_sb.t[:, tc_],
                                                           op0=ALU.mult, op1=ALU.add))
                    kb.op("act", [d_], [rn], lambda e: e.activation(out=rn.t[:, :], in_=d_.t[:, tc_], func=AF.Square))
                    ps2 = nextx()
                    kb.op("pe", [blk1, rn], [ps2], lambda e: e.matmul(ps2.t[:, :], blk1.t[:, :], rn.t[:, :], start=True, stop=True))
                    kb.op("act", [ps2], [rn],
                          lambda e: e.activation(out=rn.t[:, :], in_=ps2.t[:, :], func=AF.Sqrt, scale=1.0 / 64, bias=G["gn_eps"].t[:, 0:1]))
                    kb.op("dve", [rn], [rn], lambda e: e.reciprocal(out=rn.t[:, :], in_=rn.t[:, :]))
                    kb.op("dve", [d_, colp, rn], [d_],
                          lambda e: e.scalar_tensor_tensor(out=d_.t[:, tc_], in0=d_.t[:, tc_], scalar=cp(5), in1=rn.t[:, :],
                                                           op0=ALU.mult, op1=ALU.mult))
                    kb.op("dve", [d_, colp, bon], [d_],
                          lambda e: e.scalar_tensor_tensor(out=d_.t[:, tc_], in0=d_.t[:, tc_], scalar=cp(6), in1=bon.t[:, tc_],
                                                           op0=ALU.add, op1=ALU.add))
                    y = yo[tt % 2]
                    kb.op("dve", [d_, g_bf], [y],
                          lambda e: e.tensor_tensor(out=y.t[:, :], in0=d_.t[:, tc_], in1=g_bf.t[:, tc_], op=ALU.mult))
                    kb.dma("sp", yT_d[1536 + j * 128:1536 + (j + 1) * 128, seq_off + tt * 512: seq_off + (tt + 1) * 512], y.t[:, :],
                           r=[y], w=[ybuf])
    kb.barrier()


def yield_pair(kb, j, T, NB, NCK, At, Bt, Kt, Rt, tok, PCc, ApT_all, W_all, ArbT_all, ArkT_all, Hf, Hb_all, mN, mNT, mIT,
               ident_f, y_sb, BK, Sfree, Bfree, KtP):
    kb.barrier()
    with contextlib.ExitStack() as ec:
        NCTX = 5 if T >= 2048 else 2
        nv = T // 128
        fpool = [View(Sx.t[:, k * 128:(k + 1) * 128], f"fv{k}") for Sx in Sfree for k in range(nv)]
        bpool = [View(Bx.t[:, k * 128:(k + 1) * 128], f"bv{k}") for Bx in Bfree for k in range(nv)]
        banks = BK[0:5]
        ctx = []
        for g in range(NCTX):
            d = {}
            d["ps"] = [View(banks[g].t[:, q * 128:(q + 1) * 128], f"cps{g}_{q}", buf=banks[g].b) for q in range(4)]
            d["N"] = [fpool.pop() for _ in range(2)]
            d["M"] = [fpool.pop() for _ in range(2)]
            d["TT"] = [fpool.pop() for _ in range(2)]
            d["AakT"] = bpool.pop()
            d["W0"] = View(bpool.pop().t[:, 0:64], "cW0")
            d["TTb"] = bpool.pop()
            ctx.append(d)

        ApTv = [View(ApT_all.t[:, tb, :], f"ApTv{tb}") for tb in range(NB)]
        Wv = [View(W_all.t[:, tb, :, :], f"Wv{tb}") for tb in range(NB)]
        Arbv = [View(ArbT_all.t[:, tb, :, :], f"Arbv{tb}") for tb in range(NB)]
        Arkv = [View(ArkT_all.t[:, tb, :, :], f"Arkv{tb}") for tb in range(NB)]
        done = set()

        def cgen(d, hh, tb):
            pb = 64 * hh
            cols = slice(tb * 128, (tb + 1) * 128)
            ps = d["ps"]
            hs = slice(pb, pb + 64)
            kb.op("pe", [At, Bt], [ps[0]], lambda e: e.matmul(ps[0].t, At.t[hs, cols], Bt.t[hs, cols], start=True, stop=True))
            kb.op("pe", [At, Bt], [ps[1]], lambda e: e.matmul(ps[1].t, Bt.t[hs, cols], At.t[hs, cols], start=True, stop=True))
            kb.op("pe", [At, Kt], [ps[2]], lambda e: e.matmul(ps[2].t, Kt.t[hs, cols], At.t[hs, cols], start=True, stop=True))
            yield
            N0, M0, TT0 = d["N"][0], d["M"][0], d["TT"][0]
            kb.op("dve", [ps[0], mN], [N0], lambda e: e.tensor_tensor(out=N0.t[:, :], in0=ps[0].t, in1=mN.t[:, :], op=ALU.mult))
            kb.op("dve", [ps[1], mNT], [M0], lambda e: e.tensor_tensor(out=M0.t[:, :], in0=ps[1].t, in1=mNT.t[:, :], op=ALU.mult))
            kb.op("dve", [ps[2], mNT], [d["AakT"]],
                  lambda e: e.tensor_tensor(out=d["AakT"].t[:, :], in0=ps[2].t, in1=mNT.t[:, :], op=ALU.mult))
            kb.op(POOLENG, [M0, ident_f], [TT0],
                  lambda e: e.tensor_tensor(out=TT0.t[:, :], in0=M0.t[:, :], in1=ident_f.t[:, :], op=ALU.add))
            yield
            kb.op("pe", [Rt, Bt], [ps[3]], lambda e: e.matmul(ps[3].t, Bt.t[hs, cols], Rt.t[hs, cols], start=True, stop=True))
            kb.op("pe", [Rt, Kt], [ps[0]], lambda e: e.matmul(ps[0].t, Kt.t[hs, cols], Rt.t[hs, cols], start=True, stop=True))
            kb.op("pe", [d["AakT"], tok], [ps[1]],
                  lambda e: e.matmul(ps[1].t[:, 0:64], d["AakT"].t[:, :], tok.t[:, tb, 384 + pb:384 + pb + 64], start=True, stop=True))
            yield
            kb.op("dve", [ps[3], mIT], [Arbv[tb]],
                  lambda e: e.tensor_tensor(out=ArbT_all.t[:, tb, hh, :], in0=ps[3].t, in1=mIT.t[:, :], op=ALU.mult))
            kb.op("dve", [ps[0], mIT], [Arkv[tb]],
                  lambda e: e.tensor_tensor(out=ArkT_all.t[:, tb, hh, :], in0=ps[0].t, in1=mIT.t[:, :], op=ALU.mult))
            kb.op("act", [ps[1]], [d["W0"]], lambda e: e.activation(out=d["W0"].t[:, :], in_=ps[1].t[:, 0:64], func=AF.Copy))
            yield
            cur = 0
            for jj in range(1, 6):
                Np, Mp, TTp = d["N"][cur], d["M"][cur], d["TT"][cur]
                Nn, Mn, TTn = d["N"][1 - cur], d["M"][1 - cur], d["TT"][1 - cur]
                kb.op("pe", [Np, Mp], [ps[2]], lambda e: e.matmul(ps[2].t, Mp.t[:, :], Np.t[:, :], start=True, stop=True))
                if jj < 5:
                    kb.op("pe", [Np, Mp], [ps[3]], lambda e: e.matmul(ps[3].t, Np.t[:, :], Mp.t[:, :], start=True, stop=True))
                yield
                kb.op("act", [ps[2]], [Nn], lambda e: e.activation(out=Nn.t[:, :], in_=ps[2].t, func=AF.Copy))
                if jj < 5:
                    kb.op("act", [ps[3]], [Mn], lambda e: e.activation(out=Mn.t[:, :], in_=ps[3].t, func=AF.Copy))
                yield
                kb.op("pe", [Nn, TTp], [ps[0]], lambda e: e.matmul(ps[0].t, Nn.t[:, :], TTp.t[:, :], start=True, stop=True))
                yield
                kb.op("dve", [ps[0], TTp], [TTn],
                      lambda e: e.tensor_tensor(out=TTn.t[:, :], in0=ps[0].t, in1=TTp.t[:, :], op=ALU.add))
                yield
                cur = 1 - cur
            TTf = d["TT"][cur]
            kb.op("act", [TTf], [d["TTb"]], lambda e: e.activation(out=d["TTb"].t[:, :], in_=TTf.t[:, :], func=AF.Copy))
            yield
            kb.op("pe", [d["TTb"], d["W0"]], [ps[1]],
                  lambda e: e.matmul(ps[1].t[:, 0:64], d["TTb"].t[:, :], d["W0"].t[:, :], start=True, stop=True))
            kb.op("pe", [d["TTb"], tok], [ps[2]],
                  lambda e: e.matmul(ps[2].t[hs, :], tok.t[:, tb, pb:pb + 64], d["TTb"].t[:, :], start=True, stop=True))
            yield
            kb.op("act", [ps[1]], [Wv[tb]], lambda e: e.activation(out=W_all.t[:, tb, hh, :], in_=ps[1].t[:, 0:64], func=AF.Copy))
            kb.op("act", [ps[2]], [ApTv[tb]], lambda e: e.activation(out=ApT_all.t[hs, tb, :], in_=ps[2].t[hs, :], func=AF.Copy))
            done.add((hh, tb))
            yield

        jobs = [(hh, tb) for tb in range(NB) for hh in range(2)]

        def c_driver():
            for g0 in range(0, len(jobs), NCTX):
                gens = [cgen(ctx[g], *jobs[g0 + g]) for g in range(min(NCTX, len(jobs) - g0))]
                while gens:
                    nxt = []
                    for g_ in gens:
                        try:
                            next(g_)
                            nxt.append(g_)
                        except StopIteration:
                            pass
                    gens = nxt
                    yield

        U_sb = [View(KtP.t[:, k * 64:(k + 1) * 64], f"U_sb{k}") for k in range(2)]
        kb.op("dve", [], [Hf], lambda e: e.memset(Hf.t[:, :], 0.0))
        kb.op("dve", [], [Hb_all], lambda e: e.memset(Hb_all.t[:, 0, :], 0.0))
        HfV = [View(Hf.t[64 * hh:64 * hh + 64, :], f"Hf{hh}") for hh in range(2)]
        HbV = [View(Hb_all.t[64 * hh:64 * hh + 64, :, :], f"Hb{hh}") for hh in range(2)]
        for hh in range(2):
            HfV[hh].b.writers = dict(Hf.b.writers)
            HbV[hh].b.writers = dict(Hb_all.b.writers)

        def dgen(hh):
            pb = 64 * hh
            hs = slice(pb, pb + 64)
            bk = BK[5 + hh]
            psU = View(bk.t[:, 0:64], f"psU{hh}", buf=bk.b)
            psH = View(bk.t[:, 128:192], f"psH{hh}", buf=bk.b)
            psY = View(bk.t[:, 256:384], f"psY{hh}", buf=bk.b)
            Us = U_sb[hh]
            for tb in range(NB):
                while (hh, tb) not in done:
                    yield
                for cc in range(2):
                    ck = 2 * tb + cc
                    tp = slice(64 * cc, 64 * cc + 64)
                    kb.op("pe", [ApTv[tb], HbV[hh]], [psU],
                          lambda e: e.matmul(psU.t[tp, :], ApT_all.t[hs, tb, 64 * cc:64 * cc + 64], Hb_all.t[hs, ck, :],
                                             start=True, stop=True))
                    yield
                    kb.op("dve", [psU, Wv[tb]], [Us],
                          lambda e: e.tensor_tensor(out=Us.t[tp, :], in0=psU.t[tp, :], in1=W_all.t[tp, tb, hh, :], op=ALU.add))
                    yield
                    kb.op("pe", [tok], [psH],
                          lambda e: e.matmul(psH.t[hs, :], tok.t[tp, tb, 256 + pb:256 + pb + 64], tok.t[tp, tb, 384 + pb:384 + pb + 64],
                                             start=True, stop=False))
                    kb.op("pe", [tok, Us], [psH],
                          lambda e: e.matmul(psH.t[hs, :], tok.t[tp, tb, 128 + pb:128 + pb + 64], Us.t[tp, :], start=False, stop=True))
                    yield
                    kb.op("dve", [psH, HfV[hh], PCc], [HfV[hh]],
                          lambda e: e.scalar_tensor_tensor(out=Hf.t[hs, :], in0=Hf.t[hs, :], scalar=PCc.t[hs, ck:ck + 1],
                                                           in1=psH.t[hs, :], op0=ALU.mult, op1=ALU.add))
                    yield
                    kb.op("act", [HfV[hh]], [HbV[hh]],
                          lambda e: e.activation(out=Hb_all.t[hs, ck + 1, :], in_=Hf.t[hs, :], func=AF.Copy))
                    yield
                cols = slice(tb * 128, (tb + 1) * 128)
                kb.op("pe", [Us, Arbv[tb]], [psY],
                      lambda e: e.matmul(psY.t[hs, :], Us.t[:, :], ArbT_all.t[:, tb, hh, :], start=True, stop=False))
                kb.op("pe", [tok, Arkv[tb]], [psY],
                      lambda e: e.matmul(psY.t[hs, :], tok.t[:, tb, 384 + pb:384 + pb + 64], ArkT_all.t[:, tb, hh, :],
                                         start=False, stop=False))
                for cc in range(2):
                    kb.op("pe", [HbV[hh], Rt], [psY],
                          lambda e: e.matmul(psY.t[hs, 64 * cc:64 * cc + 64], Hb_all.t[hs, 2 * tb + cc, :],
                                             Rt.t[hs, tb * 128 + 64 * cc: tb * 128 + 64 * cc + 64], start=False, stop=(cc == 1)))
                yield
                kb.op("act", [psY], [y_sb], lambda e: e.activation(out=y_sb.t[hs, cols], in_=psY.t[hs, :], func=AF.Copy))
                yield

        run_lockstep([c_driver(), dgen(0), dgen(1)])
    kb.barrier()


CONST_NAMES = ["c_rot_ret", "c_rot_dsa", "c_cs_ret", "c_cs_dsa", "c_ret_maskT", "c_ret_qdec", "c_ret_kdec", "c_ident",
               "c_negmask", "c_maskN", "c_maskNT", "c_maskIT", "c_blockones"]


def declare_consts(nc, HC):
    C = {}
    for n in CONST_NAMES:
        C[n] = nc.dram_tensor(n, list(HC[n].shape), F32, kind="ExternalInput").ap()
    C["ret_cd"] = HC["ret_cd"]
    return C


def make_globals(kb, es, C):
    G = {}
    G["ones_f"] = kb.sb(es, [128, 128], F32, "ones_f")
    kb.op("dve", [], [G["ones_f"]], lambda e: e.memset(G["ones_f"].t[:], 1.0))
    G["ident_bf"] = kb.sb(es, [128, 128], BF16, "ident_bf")
    kb.dma("pool", G["ident_bf"].t[:, :], C["c_ident"], r=[], w=[G["ident_bf"]])
    G["ident_f"] = kb.sb(es, [128, 128], F32, "ident_f")
    kb.dma("sp", G["ident_f"].t[:, :], C["c_ident"], r=[], w=[G["ident_f"]])
    init_eps(kb, es)
    G["gn_eps"] = kb.sb(es, [128, 1], F32, "gn_eps")
    kb.op("dve", [], [G["gn_eps"]], lambda e: e.memset(G["gn_eps"].t[:], 64e-5))
    return G


OFF_GATE = 5828


def rms_hT_cols(kb, xt, gcol, hT, hcol0, sqs, ps_ss, rs, G, W):
    ones = G["ones_f"]
    for c in range(8):
        sq = sqs[c % 2]
        kb.op("act", [xt], [sq], lambda e: e.activation(out=sq.t[:, :W], in_=xt.t[:, c, :W], func=AF.Square))
        kb.op("pe", [sq, ones], [ps_ss],
              lambda e: e.matmul(ps_ss.t[:, :W], ones.t[:], sq.t[:, :W], start=(c == 0), stop=(c == 7)))
    kb.op("act", [ps_ss], [rs],
          lambda e: e.activation(out=rs.t[:, :W], in_=ps_ss.t[:, :W], func=AF.Sqrt, scale=1.0 / D, bias=eps_ap(kb)))
    kb.op("dve", [rs], [rs], lambda e: e.reciprocal(out=rs.t[:, :W], in_=rs.t[:, :W]))
    for c in range(8):
        kb.op("dve", [xt, rs, gcol], [hT],
              lambda e: e.scalar_tensor_tensor(out=hT.t[:, c, hcol0:hcol0 + W], in0=xt.t[:, c, :W], scalar=gcol.t[:, c:c + 1],
                                               in1=rs.t[:, :W], op0=ALU.mult, op1=ALU.mult))


def stage_hT(kb, xT_d, xbufs, tile0, T, g_d, hT, G):
    with contextlib.ExitStack() as es:
        gcol = kb.sb(es, [128, 8], F32, "gcol")
        kb.dma("sp", gcol.t[:, :], g_d.rearrange("(c p) -> p c", p=128), r=[], w=[gcol], allow_slow_non_contiguous=True)
        xts = [kb.sb(es, [128, 8, 512], F32, "xt") for _ in range(2)]
        sqs = [kb.sb(es, [128, 512], F32, "sq") for _ in range(2)]
        rs = kb.sb(es, [128, 512], F32, "rs")
        ps_ss = kb.ps(es, [128, 512], F32, "ps_ss")
        for tt in range(T // 512):
            xt = xts[tt % 2]
            ti = tile0 + tt
            kb.dma("sp", xt.t[:, :, :], xT_d[:, :, ti * 512:(ti + 1) * 512], r=[xbufs[ti]], w=[xt])
            rms_hT_cols(kb, xt, gcol, hT, tt * 512, sqs, ps_ss, rs, G, 512)
    kb.barrier()


def stage_merge(kb, hT, T, tile0, xT_d, xbufs, yT_d, ybuf, w_d, p_ret_d, p_dsa_d, p_rwkv_d, w_out_d, G):
    with contextlib.ExitStack() as es:
        wg = kb.sb(es, [128, 8, 3072], BF16, "wgate")
        wgs = [wg, wg, wg]

        def load_gate(s3):
            for c in range(8):
                kb.dma("pool", wg.t[:, c, s3 * 1024:(s3 + 1) * 1024],
                       w_d[c * 128:(c + 1) * 128, OFF_GATE + s3 * 1024:OFF_GATE + (s3 + 1) * 1024], r=[], w=[wg])

        load_gate(0)
        pr = kb.sb(es, [128, 8, 1024], BF16, "p_ret")
        for c in range(8):
            kb.dma("pool", pr.t[:, c, :], p_ret_d[c * 128:(c + 1) * 128, :], r=[], w=[pr])
        load_gate(1)
        pd = kb.sb(es, [64, 8, 1024], BF16, "p_dsa")
        for h in range(8):
            kb.dma("pool", pd.t[:, h, :], p_dsa_d[h * 64:(h + 1) * 64, :], r=[], w=[pd])
        load_gate(2)
        pw = kb.sb(es, [128, 4, 1024], BF16, "p_rwkv")
        for c in range(4):
            kb.dma("pool", pw.t[:, c, :], p_rwkv_d[c * 128:(c + 1) * 128, :], r=[], w=[pw])
        wo = kb.sb(es, [128, 8, 1024], BF16, "w_out")
        for c in range(8):
            kb.dma("pool", wo.t[:, c, :], w_out_d[c * 128:(c + 1) * 128, :], r=[], w=[wo])
        yr = [kb.sb(es, [128, 8, 512], BF16, "yr") for _ in range(2)]
        yd = [kb.sb(es, [64, 8, 512], BF16, "yd")] * 2
        yw = [kb.sb(es, [128, 4, 512], BF16, "yw")] * 2
        xts = [kb.sb(es, [128, 8, 512], F32, "xt")] * 2
        mg = kb.sb(es, [128, 8, 512], BF16, "merged")
        sgs = [kb.sb(es, [128, 512], F32, "sg") for _ in range(2)]
        macc = kb.sb(es, [128, 512], F32, "macc")
        tmp = kb.sb(es, [128, 512], F32, "tmp")
        psg = [kb.ps(es, [128, 512], F32, "psg") for _ in range(2)]
        psp = [kb.ps(es, [128, 512], F32, "psp") for _ in range(2)]
        pso = [kb.ps(es, [128, 512], F32, "pso") for _ in range(2)]
        it = 0
        for tt in range(T // 512):
            ti = tile0 + tt
            gcols = slice(ti * 512, (ti + 1) * 512)
            hc = slice(tt * 512, (tt + 1) * 512)
            a, b_, c_, xt = yr[tt % 2], yd[tt % 2], yw[tt % 2], xts[tt % 2]
            kb.dma("sp", a.t[:, :, :], yT_d[0:1024, gcols].rearrange("(c p) t -> p c t", p=128), r=[ybuf], w=[a])
            kb.dma("sp", b_.t[:, :, :], yT_d[1024:1536, gcols].rearrange("(h d) t -> d h t", d=64), r=[ybuf], w=[b_])
            kb.dma("sp", c_.t[:, :, :], yT_d[1536:2048, gcols].rearrange("(c p) t -> p c t", p=128), r=[ybuf], w=[c_])
            kb.dma("sp", xt.t[:, :, :], xT_d[:, :, gcols], r=[xbufs[ti]], w=[xt])
            for db in range(8):
                dsl = slice(db * 128, (db + 1) * 128)
                for br in range(3):
                    it += 1
                    pg, pp, sg = psg[it % 2], psp[it % 2], sgs[it % 2]
                    kb.op("pe", [wg, hT], [pg],
                          lambda e: [e.matmul(pg.t[:, :], wg.t[:, c, br * 1024 + db * 128: br * 1024 + (db + 1) * 128],
                                              hT.t[:, c, hc], start=(c == 0), stop=(c == 7)) for c in range(8)][-1])
                    kb.op("act", [pg], [sg], lambda e: e.activation(out=sg.t[:, :], in_=pg.t[:, :], func=AF.Sigmoid))
                    if br == 0:
                        kb.op("pe", [pr, a], [pp],
                              lambda e: [e.matmul(pp.t[:, :], pr.t[:, c, dsl], a.t[:, c, :], start=(c == 0), stop=(c == 7))
                                         for c in range(8)][-1])
                    elif br == 1:
                        kb.op("pe", [pd, b_], [pp],
                              lambda e: [e.matmul(pp.t[:, :], pd.t[:, c, dsl], b_.t[:, c, :], start=(c == 0), stop=(c == 7))
                                         for c in range(8)][-1])
                    else:
                        kb.op("pe", [pw, c_], [pp],
                              lambda e: [e.matmul(pp.t[:, :], pw.t[:, c, dsl], c_.t[:, c, :], start=(c == 0), stop=(c == 3))
                                         for c in range(4)][-1])
                    if br == 0:
                        kb.op("dve", [sg, pp], [macc],
                              lambda e: e.tensor_tensor(out=macc.t[:, :], in0=sg.t[:, :], in1=pp.t[:, :], op=ALU.mult))
                    else:
                        kb.op("dve", [sg, pp], [tmp],
                              lambda e: e.tensor_tensor(out=tmp.t[:, :], in0=sg.t[:, :], in1=pp.t[:, :], op=ALU.mult))
                        if br == 1:
                            kb.op("dve", [tmp, macc], [macc],
                                  lambda e: e.tensor_tensor(out=macc.t[:, :], in0=macc.t[:, :], in1=tmp.t[:, :], op=ALU.add))
                        else:
                            kb.op("dve", [tmp, macc], [mg],
                                  lambda e: e.tensor_tensor(out=mg.t[:, db, :], in0=macc.t[:, :], in1=tmp.t[:, :], op=ALU.add))
            for ob in range(8):
                po = pso[ob % 2]
                kb.op("pe", [wo, mg], [po],
                      lambda e: [e.matmul(po.t[:, :], wo.t[:, db, ob * 128:(ob + 1) * 128], mg.t[:, db, :],
                                          start=(db == 0), stop=(db == 7)) for db in range(8)][-1])
                kb.op("dve", [po, xt], [xt],
                      lambda e: e.tensor_tensor(out=xt.t[:, ob, :], in0=po.t[:, :], in1=xt.t[:, ob, :], op=ALU.add))
            kb.dma("sp", xT_d[:, :, gcols], xt.t[:, :, :], r=[xt], w=[xbufs[ti]])
    kb.barrier()


def stage_in(kb, x_d, xT_d, xbufs, NT, G):
    ident = G["ident_f"]
    with contextlib.ExitStack() as es:
        xin = [kb.sb(es, [128, 1024], F32, "xin") for _ in range(2)]
        xo = [kb.sb(es, [128, 8, 128], F32, "xo") for _ in range(2)]
        pst = [kb.ps(es, [128, 512], F32, "pst") for _ in range(2)]
        for tb in range(NT // 128):
            xi, xo_ = xin[tb % 2], xo[tb % 2]
            kb.dma("sp", xi.t[:, :], x_d[tb * 128:(tb + 1) * 128, :], r=[], w=[xi])
            for half in range(2):
                pt = pst[half]
                for q in range(4):
                    c = half * 4 + q
                    kb.op("pe", [xi, ident], [pt],
                          lambda e: e.transpose(pt.t[:, q * 128:(q + 1) * 128], xi.t[:, c * 128:(c + 1) * 128], ident.t[:, :]))
                kb.op("act" if half == 0 else "dve", [pt], [xo_],
                      (lambda e: e.activation(out=xo_.t[:, 0:4, :].rearrange("p a b -> p (a b)"), in_=pt.t[:, :], func=AF.Copy))
                      if half == 0 else
                      (lambda e: e.tensor_copy(out=xo_.t[:, 4:8, :].rearrange("p a b -> p (a b)"), in_=pt.t[:, :])))
            kb.dma("sp", xT_d[:, :, tb * 128:(tb + 1) * 128], xo_.t[:, :, :], r=[xo_], w=[xbufs[tb // 4]])
    kb.barrier()


def stage_out(kb, xT_d, xbufs, NT, g_d, out_d, G):
    ident = G["ident_f"]
    with contextlib.ExitStack() as es:
        gcol = kb.sb(es, [128, 8], F32, "gcol")
        kb.dma("sp", gcol.t[:, :], g_d.rearrange("(c p) -> p c", p=128), r=[], w=[gcol], allow_slow_non_contiguous=True)
        xts = [kb.sb(es, [128, 8, 512], F32, "xt") for _ in range(2)]
        sqs = [kb.sb(es, [128, 512], F32, "sq") for _ in range(2)]
        rs = kb.sb(es, [128, 512], F32, "rs")
        hf = kb.sb(es, [128, 8, 512], F32, "hf")
        ot = [kb.sb(es, [128, 1024], F32, "ot") for _ in range(2)]
        ps_ss = kb.ps(es, [128, 512], F32, "ps_ss")
        pst = [kb.ps(es, [128, 512], F32, "pst") for _ in range(2)]
        for ti in range(NT // 512):
            xt = xts[ti % 2]
            kb.dma("sp", xt.t[:, :, :], xT_d[:, :, ti * 512:(ti + 1) * 512], r=[xbufs[ti]], w=[xt])
            rms_hT_cols(kb, xt, gcol, hf, 0, sqs, ps_ss, rs, G, 512)
            for tb in range(4):
                o_ = ot[tb % 2]
                for half in range(2):
                    pt = pst[half]
                    for q in range(4):
                        c = half * 4 + q
                        kb.op("pe", [hf, ident], [pt],
                              lambda e: e.transpose(pt.t[:, q * 128:(q + 1) * 128], hf.t[:, c, tb * 128:(tb + 1) * 128], ident.t[:, :]))
                    if half == 0:
                        kb.op("act", [pt], [o_], lambda e: e.activation(out=o_.t[:, 0:512], in_=pt.t[:, :], func=AF.Copy))
                    else:
                        kb.op("dve", [pt], [o_], lambda e: e.tensor_copy(out=o_.t[:, 512:1024], in_=pt.t[:, :]))
                r0 = ti * 512 + tb * 128
                kb.dma("sp", out_d[r0:r0 + 128, :], o_.t[:, :], r=[o_], w=[])
    kb.barrier()


RWKV_NAMES = ['rwkv_mu', 'rwkv_w0', 'rwkv_w2', 'rwkv_a0', 'rwkv_a2', 'rwkv_g2', 'rwkv_k_k', 'rwkv_k_a', 'rwkv_r_k', 'rwkv_ln_w',
              'rwkv_ln_b']
W_SHAPES = {
    'norm_ffa': [1024], 'ffa_w_in': [1024, 5632], 'ffa_w_out': [2816, 1024], 'norm_mix': [1024], 'w_in': [1024, 8900],
    'rwkv_mu': [1792], 'rwkv_w0': [512], 'rwkv_w2': [64, 512], 'rwkv_a0': [512], 'rwkv_a2': [64, 512], 'rwkv_g2': [128, 512],
    'rwkv_k_k': [512], 'rwkv_k_a': [512], 'rwkv_r_k': [512], 'rwkv_ln_w': [512], 'rwkv_ln_b': [512],
    'p_ret': [1024, 1024], 'p_dsa': [512, 1024], 'p_rwkv': [512, 1024], 'w_out': [1024, 1024], 'norm_ffb': [1024],
    'ffb_w_in': [1024, 5632], 'ffb_w_out': [2816, 1024],
}


def build_program(T, NSEQ, DEPTH, HC, debug_y=False):
    NT = NSEQ * T
    nc = bass.Bass("TRN2", target_bir_lowering=False)
    x_d = nc.dram_tensor("x", [NT, 1024], F32, kind="ExternalInput").ap()
    WD = {}
    for n, shp in W_SHAPES.items():
        WD[n] = nc.dram_tensor(n, [DEPTH] + shp, F32, kind="ExternalInput").ap()
    gfin = nc.dram_tensor("norm_final", [1024], F32, kind="ExternalInput").ap()
    C = {}
    for n in CONST_NAMES:
        C[n] = nc.dram_tensor(n, list(HC[n].shape), F32, kind="ExternalInput").ap()
    C["ret_cd"] = HC["ret_cd"]
    out_d = nc.dram_tensor("out", [NT, 1024], F32, kind="ExternalOutput").ap()
    xT_t = nc.dram_tensor("xT_scr", [1024, NT], F32, kind="Internal").ap()
    if debug_y:
        yT_d = nc.dram_tensor("yT_scr", [2048, NT], BF16, kind="ExternalOutput").ap()
    else:
        yT_d = nc.dram_tensor("yT_scr", [2048, NT], BF16, kind="Internal").ap()
    xT_d = xT_t.rearrange("(c p) t -> p c t", p=128)
    kb = KB(nc)
    with kb.root:
        es = kb.root
        G = make_globals(kb, es, C)
        xbufs = [Buf(f"xb{i}") for i in range(NT // 512)]
        ybuf = Buf("ybuf")
        stage_in(kb, x_d, xT_d, xbufs, NT, G)
        for l in range(DEPTH):
            stage_ffn(kb, xT_d, xbufs, NT, WD['ffa_w_in'][l], WD['ffa_w_out'][l], WD['norm_ffa'][l], G)
            PL = {n: WD[n][l] for n in RWKV_NAMES}
            for s in range(NSEQ):
                with contextlib.ExitStack() as em:
                    hT = kb.sb(em, [128, 8, T], BF16, "hT")
                    stage_hT(kb, xT_d, xbufs, s * (T // 512), T, WD['norm_mix'][l], hT, G)
                    stage_retention(kb, hT, T, s * T, WD['w_in'][l], yT_d, ybuf, C, G)
                    stage_dsa(kb, hT, T, s * T, WD['w_in'][l], yT_d, ybuf, C, G)
                    stage_rwkv(kb, hT, T, s * T, WD['w_in'][l], PL, yT_d, ybuf, C, G)
                    stage_merge(kb, hT, T, s * (T // 512), xT_d, xbufs, yT_d, ybuf, WD['w_in'][l], WD['p_ret'][l],
                                WD['p_dsa'][l], WD['p_rwkv'][l], WD['w_out'][l], G)
                kb.barrier()
            stage_ffn(kb, xT_d, xbufs, NT, WD['ffb_w_in'][l], WD['ffb_w_out'][l], WD['norm_ffb'][l], G)
        stage_out(kb, xT_d, xbufs, NT, gfin, out_d, G)
        kb.finish()
    return nc, kb


def kernel(**inputs):
    T, NSEQ, DEPTH, NCORE = 2048, 2, 2, 8
    HC = host_consts(T)
    nc, kb = build_program(T, NSEQ, DEPTH, HC)
    shared = {}
    for n, shp in W_SHAPES.items():
        shared[n] = np.ascontiguousarray(np.asarray(inputs[n], dtype=np.float32)).reshape([DEPTH] + shp)
    shared["norm_final"] = np.ascontiguousarray(np.asarray(inputs["norm_final"], dtype=np.float32))
    for n in CONST_NAMES:
        shared[n] = HC[n]
    x = np.asarray(inputs["x"], dtype=np.float32)
    in_maps = []
    for c in range(NCORE):
        m = dict(shared)
        m["x"] = np.ascontiguousarray(x[c * NSEQ:(c + 1) * NSEQ]).reshape(NSEQ * T, 1024)
        in_maps.append(m)
    res = run_bass_kernel_spmd(nc, in_maps, core_ids=list(range(NCORE)))
    outs = [np.asarray(r["out"], dtype=np.float32).reshape(NSEQ, T, 1024) for r in res.results]
    return np.concatenate(outs, axis=0)
```
